# Optimizing a Trainium2 kernel written in Bass

```python
import math
import jax, jax.numpy as jnp
from jax import lax
import numpy as np

D_MODEL = 4096
BATCH = 4
SEQ = 2048
DEPTH = 1

HEAD_DIM = 128
ROT_DIV = 4
ROPE_THETA = 500000.0
EPS = 1e-6
A_HEADS = D_MODEL // 256
A_KV_HEADS = A_HEADS // 4
A_GROUP = A_HEADS // A_KV_HEADS
IDX_HEADS = 16
IDX_DIM = 64
TOPK_MAX = 256
A_QBLOCK = 64
B_HEADS = D_MODEL // 512
B_QBLOCK = 128
N_GROUPS = 4
EXPERTS_PER_GROUP = 8
N_EXPERTS = N_GROUPS * EXPERTS_PER_GROUP
EXPERT_TOPK = 2
D_EXPERT = (3 * D_MODEL) // 8
MOE_BLOCK = 256

A_Q = A_HEADS * HEAD_DIM
A_KV = A_KV_HEADS * HEAD_DIM
IDX_Q = IDX_HEADS * IDX_DIM
B_QK = 2 * B_HEADS * HEAD_DIM
B_V = B_HEADS * 2 * HEAD_DIM
IN_SPLITS = [A_Q, A_KV, A_KV, IDX_Q, IDX_DIM, IDX_HEADS, B_QK, B_QK, B_V, D_MODEL, D_MODEL]
D_IN = sum(IN_SPLITS)
IN_CUTS = [int(v) for v in np.cumsum(IN_SPLITS)[:-1]]

kernel_name = "hybrid_dsa_diffattn_hmoe_block"


def rms_norm(x, g):
    xf = x.astype(jnp.float32)
    y = xf * lax.rsqrt(jnp.mean(xf * xf, axis=-1, keepdims=True) + EPS)
    return (y * g.astype(jnp.float32)).astype(x.dtype)


def modulate(h, shift, scale):
    return h * (1.0 + scale[:, None, :]) + shift[:, None, :]


def rope_tables(positions, dim):
    rot = dim // ROT_DIV
    inv = 1.0 / (ROPE_THETA ** (jnp.arange(0, rot, 2, dtype=jnp.float32) / rot))
    ang = positions.astype(jnp.float32)[..., None] * inv
    return jnp.cos(ang), jnp.sin(ang)


def apply_partial_rope(t, cos, sin):
    half = cos.shape[-1]
    rot = 2 * half
    c = cos[:, :, None, :].astype(t.dtype)
    s = sin[:, :, None, :].astype(t.dtype)
    x1 = t[..., :half]
    x2 = t[..., half:rot]
    return jnp.concatenate([x1 * c - x2 * s, x2 * c + x1 * s, t[..., rot:]], axis=-1)


def to_blocks(t, qb):
    b, s = t.shape[:2]
    return t.reshape((b, s // qb, qb) + t.shape[2:]).swapaxes(0, 1)


def sparse_indexer_attention(q, k, v, qi, ki, wi, cos, sin, cos_i, sin_i):
    b, s = q.shape[:2]
    q = apply_partial_rope(q.reshape(b, s, A_HEADS, HEAD_DIM), cos, sin) * (HEAD_DIM ** -0.5)
    q = q.reshape(b, s, A_KV_HEADS, A_GROUP, HEAD_DIM)
    k = apply_partial_rope(k.reshape(b, s, A_KV_HEADS, HEAD_DIM), cos, sin)
    v = v.reshape(b, s, A_KV_HEADS, HEAD_DIM)
    qi = apply_partial_rope(qi.reshape(b, s, IDX_HEADS, IDX_DIM), cos_i, sin_i) * (IDX_DIM ** -0.5)
    ki = apply_partial_rope(ki[:, :, None, :], cos_i, sin_i)[:, :, 0, :]
    wi = wi * (IDX_HEADS ** -0.5)
    topk = min(TOPK_MAX, s // 4)
    nb = s // A_QBLOCK
    key_pos = jnp.arange(s)

    def body(args):
        qb, qib, wib, i = args
        t = i * A_QBLOCK + jnp.arange(A_QBLOCK)
        rel = jax.nn.relu(jnp.einsum('bqhd,bsd->bqhs', qib, ki).astype(jnp.float32))
        score = jnp.einsum('bqh,bqhs->bqs', wib.astype(jnp.float32), rel)
        causal = key_pos[None, :] <= t[:, None]
        score = jnp.where(causal[None], score, -jnp.inf)
        _, idx = lax.top_k(score, topk)
        valid = idx <= t[None, :, None]
        ks = jax.vmap(lambda kk, ii: kk[ii])(k, idx)
        vs = jax.vmap(lambda vv, ii: vv[ii])(v, idx)
        sc = jnp.einsum('bqhgd,bqkhd->bqhgk', qb, ks).astype(jnp.float32)
        sc = jnp.where(valid[:, :, None, None, :], sc, -jnp.inf)
        p = jax.nn.softmax(sc, axis=-1).astype(vs.dtype)
        o = jnp.einsum('bqhgk,bqkhd->bqhgd', p, vs)
        return o.reshape(b, A_QBLOCK, A_Q)

    out = lax.map(body, (to_blocks(q, A_QBLOCK), to_blocks(qi, A_QBLOCK), to_blocks(wi, A_QBLOCK), jnp.arange(nb)))
    return out.swapaxes(0, 1).reshape(b, s, A_Q)


def differential_attention(q, k, v, cos, sin, lam_q1, lam_k1, lam_q2, lam_k2, g_subln, lambda_init):
    b, s = q.shape[:2]
    q = apply_partial_rope(q.reshape(b, s, 2 * B_HEADS, HEAD_DIM), cos, sin) * (HEAD_DIM ** -0.5)
    k = apply_partial_rope(k.reshape(b, s, 2 * B_HEADS, HEAD_DIM), cos, sin)
    v = v.reshape(b, s, B_HEADS, 2 * HEAD_DIM)
    lam = (jnp.exp(jnp.sum(lam_q1.astype(jnp.float32) * lam_k1.astype(jnp.float32)))
           - jnp.exp(jnp.sum(lam_q2.astype(jnp.float32) * lam_k2.astype(jnp.float32))) + lambda_init)
    nb = s // B_QBLOCK
    key_pos = jnp.arange(s)

    def body(args):
        qb, i = args
        t = i * B_QBLOCK + jnp.arange(B_QBLOCK)
        sc = jnp.einsum('bqhd,bkhd->bhqk', qb, k).astype(jnp.float32)
        causal = key_pos[None, :] <= t[:, None]
        sc = jnp.where(causal[None, None], sc, -jnp.inf)
        p = jax.nn.softmax(sc, axis=-1).reshape(b, B_HEADS, 2, B_QBLOCK, s)
        a = p[:, :, 0] - lam * p[:, :, 1]
        o = jnp.einsum('bhqk,bkhe->bqhe', a.astype(v.dtype), v)
        o = rms_norm(o, g_subln) * (1.0 - lambda_init)
        return o.reshape(b, B_QBLOCK, B_V)

    out = lax.map(body, (to_blocks(q, B_QBLOCK), jnp.arange(nb)))
    return out.swapaxes(0, 1).reshape(b, s, B_V)


def hierarchical_moe(h, router_g, router_g_b, router_e, router_e_b, w_gate, w_up, w_down):
    b, s, d = h.shape
    n = b * s
    hf = h.reshape(n, d)
    pg = jax.nn.softmax((hf @ router_g).astype(jnp.float32) + router_g_b.astype(jnp.float32), axis=-1)
    g_top, g_idx = lax.top_k(pg, 1)
    el = jnp.einsum('nd,gde->nge', hf, router_e).astype(jnp.float32) + router_e_b.astype(jnp.float32)
    el_sel = jnp.take_along_axis(el, g_idx[:, :, None], axis=1)[:, 0]
    e_val, e_idx = lax.top_k(el_sel, EXPERT_TOPK)
    pe = jax.nn.softmax(e_val, axis=-1)
    weights = (g_top * pe).reshape(-1)
    expert = (g_idx * EXPERTS_PER_GROUP + e_idx).reshape(-1)
    token = jnp.repeat(jnp.arange(n, dtype=jnp.int32), EXPERT_TOPK)
    a_tot = n * EXPERT_TOPK
    p_tot = ((a_tot + N_EXPERTS * (MOE_BLOCK - 1) + MOE_BLOCK - 1) // MOE_BLOCK) * MOE_BLOCK
    nblk = p_tot // MOE_BLOCK
    order = jnp.argsort(expert)
    sorted_e = expert[order]
    counts = jnp.zeros((N_EXPERTS,), jnp.int32).at[expert].add(1)
    starts = jnp.cumsum(counts) - counts
    padded = ((counts + MOE_BLOCK - 1) // MOE_BLOCK) * MOE_BLOCK
    pends = jnp.cumsum(padded)
    pstarts = pends - padded
    rank = jnp.arange(a_tot, dtype=jnp.int32) - starts[sorted_e]
    dest = pstarts[sorted_e] + rank
    slot_token = jnp.zeros((p_tot,), jnp.int32).at[dest].set(token[order])
    slot_w = jnp.zeros((p_tot,), jnp.float32).at[dest].set(weights[order])
    block_expert = jnp.clip(jnp.searchsorted(pends, jnp.arange(nblk, dtype=jnp.int32) * MOE_BLOCK, side='right'), 0, N_EXPERTS - 1)

    def expert_block(args):
        tok, wt, e = args
        xb = hf[tok]
        y = (jax.nn.silu(xb @ w_gate[e]) * (xb @ w_up[e])) @ w_down[e]
        return y * wt[:, None].astype(y.dtype)

    ys = lax.map(expert_block, (slot_token.reshape(nblk, MOE_BLOCK), slot_w.reshape(nblk, MOE_BLOCK), block_expert))
    out = jnp.zeros((n, d), ys.dtype).at[slot_token].add(ys.reshape(p_tot, d))
    return out.reshape(b, s, d)


def setup_inputs(seed: int = 0) -> dict:
    key = jax.random.key(seed)
    ks = jax.random.split(key, 26)
    f32 = jnp.float32
    nrm = lambda k, shape, sc: jax.random.normal(k, shape, f32) * sc
    x = nrm(ks[0], (BATCH, SEQ, D_MODEL), 1.0)
    c = nrm(ks[1], (BATCH, D_MODEL), 1.0)
    start = jax.random.randint(ks[2], (BATCH, 1), 0, 4096, dtype=jnp.int32)
    positions = start + jnp.arange(SEQ, dtype=jnp.int32)[None, :]
    return {
        'x': x,
        'c': c,
        'positions': positions,
        'w_ada': nrm(ks[3], (DEPTH, D_MODEL, 6 * D_MODEL), 0.5 * D_MODEL ** -0.5),
        'b_ada': nrm(ks[4], (DEPTH, 6 * D_MODEL), 0.01),
        'g_norm1': 1.0 + nrm(ks[5], (DEPTH, D_MODEL), 0.05),
        'w_in': nrm(ks[6], (DEPTH, D_MODEL, D_IN), D_MODEL ** -0.5),
        'lam_q1': nrm(ks[7], (DEPTH, HEAD_DIM), 0.1),
        'lam_k1': nrm(ks[8], (DEPTH, HEAD_DIM), 0.1),
        'lam_q2': nrm(ks[9], (DEPTH, HEAD_DIM), 0.1),
        'lam_k2': nrm(ks[10], (DEPTH, HEAD_DIM), 0.1),
        'g_subln': 1.0 + nrm(ks[11], (DEPTH, 2 * HEAD_DIM), 0.05),
        'w_proj_a': nrm(ks[12], (DEPTH, A_Q, D_MODEL), A_Q ** -0.5),
        'w_proj_b': nrm(ks[13], (DEPTH, B_V, D_MODEL), B_V ** -0.5),
        'w_out': nrm(ks[14], (DEPTH, D_MODEL, D_MODEL), D_MODEL ** -0.5),
        'g_norm2': 1.0 + nrm(ks[15], (DEPTH, D_MODEL), 0.05),
        'router_g': nrm(ks[16], (DEPTH, D_MODEL, N_GROUPS), D_MODEL ** -0.5),
        'router_g_b': nrm(ks[17], (DEPTH, N_GROUPS), 0.01),
        'router_e': nrm(ks[18], (DEPTH, N_GROUPS, D_MODEL, EXPERTS_PER_GROUP), D_MODEL ** -0.5),
        'router_e_b': nrm(ks[19], (DEPTH, N_GROUPS, EXPERTS_PER_GROUP), 0.01),
        'w_gate': nrm(ks[20], (DEPTH, N_EXPERTS, D_MODEL, D_EXPERT), D_MODEL ** -0.5),
        'w_up': nrm(ks[21], (DEPTH, N_EXPERTS, D_MODEL, D_EXPERT), D_MODEL ** -0.5),
        'w_down': nrm(ks[22], (DEPTH, N_EXPERTS, D_EXPERT, D_MODEL), D_EXPERT ** -0.5),
        'g_final': 1.0 + nrm(ks[23], (D_MODEL,), 0.05),
    }


def reference(x, c, positions, w_ada, b_ada, g_norm1, w_in, lam_q1, lam_k1, lam_q2, lam_k2, g_subln,
              w_proj_a, w_proj_b, w_out, g_norm2, router_g, router_g_b, router_e, router_e_b,
              w_gate, w_up, w_down, g_final):
    cos, sin = rope_tables(positions, HEAD_DIM)
    cos_i, sin_i = rope_tables(positions, IDX_DIM)
    c_act = jax.nn.silu(c)
    for l in range(DEPTH):
        lambda_init = 0.8 - 0.6 * math.exp(-0.3 * l)
        mod = c_act @ w_ada[l] + b_ada[l]
        shift1, scale1, gate1, shift2, scale2, gate2 = jnp.split(mod, 6, axis=-1)
        h = modulate(rms_norm(x, g_norm1[l]), shift1, scale1)
        proj = h @ w_in[l]
        qa, ka, va, qi, ki, wi, qb, kb, vb, gate_a, gate_b = jnp.split(proj, IN_CUTS, axis=-1)
        o_a = sparse_indexer_attention(qa, ka, va, qi, ki, wi, cos, sin, cos_i, sin_i)
        o_b = differential_attention(qb, kb, vb, cos, sin, lam_q1[l], lam_k1[l], lam_q2[l], lam_k2[l],
                                     g_subln[l], lambda_init)
        merged = jax.nn.sigmoid(gate_a) * (o_a @ w_proj_a[l]) + jax.nn.sigmoid(gate_b) * (o_b @ w_proj_b[l])
        x = x + gate1[:, None, :] * (merged @ w_out[l])
        h = modulate(rms_norm(x, g_norm2[l]), shift2, scale2)
        y = hierarchical_moe(h, router_g[l], router_g_b[l], router_e[l], router_e_b[l],
                             w_gate[l], w_up[l], w_down[l])
        x = x + gate2[:, None, :] * y
    return rms_norm(x, g_final)
```

```python
import numpy as np
from contextlib import ExitStack
import concourse.bass as bass
import concourse.mybir as mybir
from concourse.bass_utils import run_bass_kernel_spmd

F32 = mybir.dt.float32
I32 = mybir.dt.int32
AF = mybir.ActivationFunctionType
ALU = mybir.AluOpType
AX = mybir.AxisListType
NCORES = 8
EPS = 1e-6


class Res:
    __slots__ = ("name", "lw", "rd")

    def __init__(self, name):
        self.name = name
        self.lw = None
        self.rd = {}


class Ctx:
    def __init__(self):
        self.nc = bass.Bass("TRN2", target_bir_lowering=False)
        self.es = ExitStack()
        nc = self.nc
        self.eng = {"pe": nc.tensor, "act": nc.scalar, "dve": nc.vector, "pool": nc.gpsimd, "sp": nc.sync}
        self.sems = {}
        self.cnt = {}
        self.waited = {k: {} for k in self.eng}
        for k in ("pe", "act", "dve", "pool"):
            self._sem("E_" + k)
        self.psum_i = 0

    def _sem(self, key):
        if key not in self.sems:
            self.sems[key] = self.es.enter_context(self.nc.semaphore("s_" + key))
            self.cnt[key] = 0
        return self.sems[key]

    def dram(self, name, shape, dt=F32, kind="Internal"):
        t = self.nc.dram_tensor(name, list(shape), dt, kind=kind)
        return t.ap(), Res(name)

    def sb(self, name, shape, dt=F32):
        t = self.es.enter_context(self.nc.sbuf_tensor(name, list(shape), dt))
        return t, Res(name)

    def ps(self, name, shape, dt=F32):
        t = self.es.enter_context(self.nc.psum_tensor(name, list(shape), dt))
        return t, Res(name)

    def _wait(self, en, deps, keep_last=False):
        w = self.waited[en]
        e = self.eng[en]
        need = {}
        for key, val in deps:
            if en == "pe" and key == "E_pe":
                continue
            if w.get(key, 0) < val and need.get(key, 0) < val:
                need[key] = val
        items = list(need.items())
        last = items.pop() if (keep_last and items) else None
        for key, val in items:
            e.wait_ge(self.sems[key], val)
            w[key] = val
        if last is not None:
            w[last[0]] = last[1]
        return last

    def _deps(self, reads, writes):
        deps = []
        for r in reads:
            if r.lw is not None:
                deps.append(r.lw)
        for wr in writes:
            if wr.lw is not None:
                deps.append(wr.lw)
            deps.extend(wr.rd.items())
        return deps

    def _mark(self, tok, reads, writes):
        for r in reads:
            if r.rd.get(tok[0], 0) < tok[1]:
                r.rd[tok[0]] = tok[1]
        for wr in writes:
            wr.lw = tok
            wr.rd = {}

    def op(self, en, fn, reads=(), writes=()):
        last = self._wait(en, self._deps(reads, writes), keep_last=True)
        ins = fn(self.eng[en])
        if last is not None:
            ins = ins._wait_ge(self.sems[last[0]], last[1])
        key = "E_" + en
        self.cnt[key] += 1
        ins.then_inc(self.sems[key], 1)
        self._mark((key, self.cnt[key]), reads, writes)

    def dma(self, en, out, in_, reads, writes, **kw):
        last = self._wait(en, self._deps(reads, writes), keep_last=True)
        key = "D_" + writes[0].name
        self._sem(key)
        ins = self.eng[en].dma_start(out=out, in_=in_, **kw)
        if last is not None:
            ins = ins._wait_ge(self.sems[last[0]], last[1])
        self.cnt[key] += 16
        ins.then_inc(self.sems[key], 16)
        self._mark((key, self.cnt[key]), reads, writes)

    def finish(self, outs):
        deps = []
        for r in outs:
            if r.lw is not None:
                deps.append(r.lw)
        self._wait("sp", deps)

    def run(self, in_maps):
        self.es.close()
        res = run_bass_kernel_spmd(self.nc, in_maps, core_ids=list(range(NCORES)))
        return res.results


def mm(ctx, out, lhsT, rhs, start, stop, reads, writes):
    ctx.op("pe", lambda e: e.matmul(out, lhsT, rhs, start=start, stop=stop), reads, writes)


def build_s0(D, NB, NCOL):
    KC = D // 128
    NBLK = NCOL // 512
    c = Ctx()
    cT, r_cT = c.dram("cT", [128, KC * NB], kind="ExternalInput")
    w, r_w = c.dram("w", [NBLK, 128, KC * 512], kind="ExternalInput")
    b, r_b = c.dram("b", [NB, NCOL], kind="ExternalInput")
    o, r_o = c.dram("o", [NB, NCOL], kind="ExternalOutput")
    cs, r_cs = c.sb("cs", [128, KC * NB])
    bs, r_bs = c.sb("bs", [NB, NCOL])
    os_, r_os = c.sb("os", [NB, NCOL])
    wt = [c.sb(f"wt{i}", [128, KC * 512]) for i in range(2)]
    pst = [c.ps(f"ps{i}", [128, 512]) for i in range(2)]
    c.dma("sp", cs[:], cT, [r_cT], [r_cs])
    c.dma("sp", bs[:], b, [r_b], [r_bs])
    c.op("act", lambda e: e.activation(out=cs[:], in_=cs[:], func=AF.Silu), [r_cs], [r_cs])
    for n in range(NBLK):
        wtile, r_wt = wt[n % 2]
        c.dma("sp", wtile[:], w[n], [r_w], [r_wt])
        p, r_p = pst[n % 2]
        for k in range(KC):
            mm(c, p[0:NB, :], cs[:, k * NB:(k + 1) * NB], wtile[:, k * 512:(k + 1) * 512],
               k == 0, k == KC - 1, [r_cs, r_wt], [r_p])
        c.op("dve", lambda e: e.tensor_tensor(out=os_[:, n * 512:(n + 1) * 512], in0=p[0:NB, :],
                                              in1=bs[:, n * 512:(n + 1) * 512], op=ALU.add),
             [r_p, r_bs], [r_os])
    c.dma("sp", o, os_[:], [r_os], [r_o])
    c.finish([r_o])
    return c


def run_s0(c_in, w_ada, b_ada):
    NB, D = c_in.shape
    NTOT = w_ada.shape[1]
    NCOL = NTOT // NCORES
    KC = D // 128
    ctx = build_s0(D, NB, NCOL)
    cT = np.ascontiguousarray(c_in.T.reshape(KC, 128, NB).transpose(1, 0, 2).reshape(128, KC * NB))
    maps = []
    for r in range(NCORES):
        ws = w_ada[:, r * NCOL:(r + 1) * NCOL]
        wt = ws.reshape(KC, 128, NCOL // 512, 512).transpose(2, 1, 0, 3)
        maps.append({"cT": cT, "w": np.ascontiguousarray(wt).reshape(NCOL // 512, 128, KC * 512),
                     "b": np.ascontiguousarray(np.broadcast_to(b_ada[None, r * NCOL:(r + 1) * NCOL], (NB, NCOL)))})
    res = ctx.run(maps)
    return np.concatenate([res[r]["o"] for r in range(NCORES)], axis=1)


def build_s1(D, TB, MT, TBLK=512, TPB=4):
    KC = D // 128
    NBATCH = TB // TPB
    c = Ctx()
    xT, r_xT = c.dram("xT", [TB, 128, KC * TBLK], kind="ExternalInput")
    w, r_w = c.dram("w", [MT, 128, KC * 128], kind="ExternalInput")
    g, r_g = c.dram("g", [128, KC], kind="ExternalInput")
    sc, r_sc = c.dram("sc", [128, KC * NBATCH], kind="ExternalInput")
    sh, r_sh = c.dram("sh", [128, KC * NBATCH], kind="ExternalInput")
    o, r_o = c.dram("o", [MT * 128, TB * TBLK], kind="ExternalOutput")
    gs, r_gs = c.sb("gs", [128, KC])
    A, r_A = c.sb("A", [128, KC, NBATCH])
    S, r_S = c.sb("S", [128, KC, NBATCH])
    ones, r_ones = c.sb("ones", [128, 128])
    xt = [c.sb(f"xt{i}", [128, KC, TBLK]) for i in range(2)]
    wt = [c.sb(f"wt{i}", [128, KC, 128]) for i in range(2)]
    sq = [c.sb(f"sq{i}", [128, TBLK]) for i in range(2)]
    ot = [c.sb(f"ot{i}", [128, TBLK]) for i in range(2)]
    rstd, r_rstd = c.sb("rstd", [128, TBLK])
    pss, r_pss = c.ps("pss", [128, TBLK])
    psm = [c.ps(f"psm{i}", [128, TBLK]) for i in range(2)]
    c.dma("sp", gs[:], g, [r_g], [r_gs])
    c.dma("sp", A[:].rearrange("p k b -> p (k b)"), sc, [r_sc], [r_A])
    c.dma("sp", S[:].rearrange("p k b -> p (k b)"), sh, [r_sh], [r_S])
    c.op("dve", lambda e: e.memset(ones[:], 1.0), [], [r_ones])
    c.op("dve", lambda e: e.tensor_scalar(out=A[:], in0=A[:], scalar1=1.0, scalar2=None, op0=ALU.add), [r_A], [r_A])
    for bb in range(NBATCH):
        c.op("dve", lambda e: e.tensor_tensor(out=A[:, :, bb], in0=A[:, :, bb], in1=gs[:], op=ALU.mult),
             [r_A, r_gs], [r_A])
    def load_x(tb):
        x, r_x = xt[tb % 2]
        c.dma("sp", x[:].rearrange("p k t -> p (k t)"), xT[tb], [r_xT], [r_x])

    def prep(tb):
        bb = tb // TPB
        x, r_x = xt[tb % 2]
        for k in range(KC):
            s, r_s = sq[k % 2]
            c.op("act", lambda e: e.activation(out=s[:], in_=x[:, k, :], func=AF.Square), [r_x], [r_s])
            mm(c, pss[:], ones[:], s[:], k == 0, k == KC - 1, [r_ones, r_s], [r_pss])
        c.op("dve", lambda e: e.tensor_scalar(out=rstd[:], in0=pss[:], scalar1=1.0 / D, scalar2=EPS,
                                              op0=ALU.mult, op1=ALU.add), [r_pss], [r_rstd])
        c.op("act", lambda e: e.activation(out=rstd[:], in_=rstd[:], func=AF.Sqrt), [r_rstd], [r_rstd])
        c.op("dve", lambda e: e.reciprocal(out=rstd[:], in_=rstd[:]), [r_rstd], [r_rstd])
        for k in range(KC):
            c.op("dve", lambda e: e.scalar_tensor_tensor(out=x[:, k, :], in0=x[:, k, :], scalar=A[:, k, bb:bb + 1],
                                                         in1=rstd[:], op0=ALU.mult, op1=ALU.mult),
                 [r_x, r_A, r_rstd], [r_x])
            c.op("pool", lambda e: e.tensor_scalar(out=x[:, k, :], in0=x[:, k, :], scalar1=S[:, k, bb:bb + 1],
                                                   scalar2=None, op0=ALU.add), [r_x, r_S], [r_x])

    wi = 0
    load_x(0)
    prep(0)
    for tb in range(TB):
        x, r_x = xt[tb % 2]
        if tb + 1 < TB:
            load_x(tb + 1)
        for m in range(MT):
            if m == min(3, MT - 1) and tb + 1 < TB:
                prep(tb + 1)
            wtile, r_wt = wt[wi % 2]
            c.dma("sp", wtile[:].rearrange("p k n -> p (k n)"), w[m], [r_w], [r_wt])
            p, r_p = psm[wi % 2]
            for k in range(KC):
                mm(c, p[:], wtile[:, k, :], x[:, k, :], k == 0, k == KC - 1, [r_wt, r_x], [r_p])
            oo, r_oo = ot[wi % 2]
            c.op("act", lambda e: e.activation(out=oo[:], in_=p[:], func=AF.Copy), [r_p], [r_oo])
            c.dma("pool", o[m * 128:(m + 1) * 128, tb * TBLK:(tb + 1) * TBLK], oo[:], [r_oo], [r_o])
            wi += 1
    c.finish([r_o])
    return c


def tile_w(wm, KC):
    K, N = wm.shape
    return np.ascontiguousarray(wm.reshape(KC, 128, N // 128, 128).transpose(2, 1, 0, 3)).reshape(N // 128, 128, KC * 128)


def tile_xT(xm, TBLK):
    T, D = xm.shape
    KC = D // 128
    return np.ascontiguousarray(xm.reshape(T // TBLK, TBLK, KC, 128).transpose(0, 3, 2, 1)).reshape(T // TBLK, 128, KC * TBLK)


def vec_pk(v, KC):
    v = np.asarray(v)
    lead = v.shape[:-1]
    vv = v.reshape(-1, KC, 128).transpose(2, 1, 0)
    return np.ascontiguousarray(vv).reshape(128, -1) if lead else np.ascontiguousarray(vv[:, :, 0])


def run_s1(x2d, w_in, g1, scale1, shift1, TPB):
    T, D = x2d.shape
    KC = D // 128
    N = w_in.shape[1]
    TBLK = 512
    TB = T // TBLK
    MT = -(-N // (128 * NCORES))
    NP = MT * 128 * NCORES
    wp = np.zeros((D, NP), np.float32)
    wp[:, :N] = w_in
    ctx = build_s1(D, TB, MT, TBLK, TPB)
    xT = tile_xT(x2d, TBLK)
    maps = []
    for r in range(NCORES):
        maps.append({"xT": xT, "w": tile_w(wp[:, r * MT * 128:(r + 1) * MT * 128], KC), "g": vec_pk(g1, KC),
                     "sc": vec_pk(scale1, KC), "sh": vec_pk(shift1, KC)})
    res = ctx.run(maps)
    return np.concatenate([res[r]["o"] for r in range(NCORES)], axis=0)[:N]


def build_s2(S, QB, HA, HKV, HB, HI, TOPK, LAMBDA_INIT):
    ST = S // 128
    QS = QB // 128
    NQ = 2 * QB
    GRP = HA // HKV
    SB = min(512, S)
    NSB = S // SB
    lim = [2 * QS, 4 * QS]
    c = Ctx()
    IN = lambda n, s, dt=F32: c.dram(n, s, dt, kind="ExternalInput")
    qaT, r_in = IN("qaT", [HA, 128, NQ]); kaT, _ = IN("kaT", [HKV, 128, S]); va, _ = IN("va", [HKV, 128, ST * 128])
    qiT, _ = IN("qiT", [HI, 64, NQ]); kiT, _ = IN("kiT", [64, S]); wi, _ = IN("wi", [NQ // 128, 128, HI])
    qbT, _ = IN("qbT", [2 * HB, 128, NQ]); kbT, _ = IN("kbT", [2 * HB, 128, S]); vb, _ = IN("vb", [HB, 128, ST * 256])
    posk, _ = IN("posk", [32, S], I32); posq, _ = IN("posq", [32, NQ], I32)
    inv, _ = IN("inv", [32, 2]); cb, _ = IN("cb", [NQ // 128, 128, S]); cmT, _ = IN("cmT", [2, 128, ST * QB])
    lamv, _ = IN("lamv", [128, 4 * 128]); gsub, _ = IN("gsub", [128, 256])
    r32, _ = IN("r32", [32, 32]); r16, _ = IN("r16", [16, 16]); idn, _ = IN("idn", [128, 128])
    oa, r_oa = c.dram("oa", [NQ, HA * 128], kind="ExternalOutput")
    ob, r_ob = c.dram("ob", [NQ, HB * 256], kind="ExternalOutput")

    def load(name, shape, src, dt=F32, eng="sp"):
        t, r = c.sb(name, shape, dt)
        c.dma(eng, t[:] if len(shape) == 2 else t[:].rearrange("p a b -> p (a b)"), src, [r_in], [r])
        return t, r

    inv_s, r_inv = load("inv_s", [32, 2], inv)
    R32, r_R32 = load("R32", [32, 32], r32); R16, r_R16 = load("R16", [16, 16], r16); I128, r_I = load("I128", [128, 128], idn)
    lam_s, r_lam = load("lam_s", [128, 512], lamv); gs_s, r_gs = load("gs_s", [128, 256], gsub)
    PI = float(np.pi)

    npi, r_npi = c.sb("npi", [32, 1])
    c.op("dve", lambda e: e.memset(npi[:], -PI), [], [r_npi])

    pi_, r_pi = c.sb("tb_pi", [32, 512], I32)
    pf, r_pf = c.sb("tb_pf", [32, 512])
    rr, r_rr = c.sb("tb_rr", [32, 512])
    Ck_, r_Ck = c.sb("tb_Ck", [32, S]); Sk_, r_Sk = c.sb("tb_Sk", [32, S])
    Cq_, r_Cq = c.sb("tb_Cq", [32, NQ]); Sq_, r_Sq = c.sb("tb_Sq", [32, NQ])

    def tables(pos_ap, n, col, Ct, r_C, St, r_S):
        for b0 in range(0, n, 512):
            w = min(512, n - b0)
            c.dma("sp", pi_[:, 0:w], pos_ap[:, b0:b0 + w], [r_in], [r_pi])
            c.op("dve", lambda e: e.tensor_copy(out=pf[:, 0:w], in_=pi_[:, 0:w]), [r_pi], [r_pf])
            c.op("dve", lambda e: e.tensor_scalar(out=pf[:, 0:w], in0=pf[:, 0:w], scalar1=inv_s[:, col:col + 1], scalar2=None,
                                                  op0=ALU.mult), [r_pf, r_inv], [r_pf])
            for dst, r_d, off in ((St, r_S, 0.0), (Ct, r_C, PI / 2)):
                c.op("dve", lambda e: e.tensor_scalar(out=rr[:, 0:w], in0=pf[:, 0:w], scalar1=off, scalar2=1.0 / (2 * PI),
                                                      op0=ALU.add, op1=ALU.mult), [r_pf], [r_rr])
                c.op("dve", lambda e: e.tensor_copy(out=pi_[:, 0:w], in_=rr[:, 0:w]), [r_rr], [r_pi])
                c.op("dve", lambda e: e.tensor_copy(out=rr[:, 0:w], in_=pi_[:, 0:w]), [r_pi], [r_rr])
                c.op("dve", lambda e: e.scalar_tensor_tensor(out=dst[:, b0:b0 + w], in0=rr[:, 0:w], scalar=-2 * PI, in1=pf[:, 0:w],
                                                             op0=ALU.mult, op1=ALU.add), [r_rr, r_pf], [r_d])
                if off != 0.0:
                    c.op("dve", lambda e: e.tensor_scalar(out=dst[:, b0:b0 + w], in0=dst[:, b0:b0 + w], scalar1=off, scalar2=None,
                                                          op0=ALU.add), [r_d], [r_d])
                c.op("dve", lambda e: e.tensor_scalar(out=rr[:, 0:w], in0=dst[:, b0:b0 + w], scalar1=PI, scalar2=None,
                                                      op0=ALU.is_gt), [r_d], [r_rr])
                c.op("dve", lambda e: e.scalar_tensor_tensor(out=dst[:, b0:b0 + w], in0=rr[:, 0:w], scalar=-2 * PI, in1=dst[:, b0:b0 + w],
                                                             op0=ALU.mult, op1=ALU.add), [r_rr, r_d], [r_d])
                c.op("act", lambda e: e.activation(out=dst[:, b0:b0 + w], in_=dst[:, b0:b0 + w], func=AF.Sin), [r_d], [r_d])
        return (Ct, r_C, St, r_S)

    TKI = tables(posk, S, 1, Ck_, r_Ck, Sk_, r_Sk)
    TQI = tables(posq, NQ, 1, Cq_, r_Cq, Sq_, r_Sq)

    ps_misc = [c.ps(f"psm{i}", [128, 512]) for i in range(2)]
    ps_st = [c.ps(f"pst{i}", [128, 512]) for i in range(2)]
    ps_o = [c.ps(f"pso{i}", [128, 512]) for i in range(4)]
    rt1, r_rt1 = c.sb("rt1", [32, 512]); rt2, r_rt2 = c.sb("rt2", [32, 512])
    state = {"m": 0}

    def rope(t, r_t, n, tab, rot, Rm, r_Rm):
        Ct, r_C, St, r_S = tab
        for b0 in range(0, n, 512):
            w = min(512, n - b0)
            p, r_p = ps_misc[state["m"] % 2]; state["m"] += 1
            mm(c, p[0:rot, 0:w], Rm[:, :], t[0:rot, b0:b0 + w], True, True, [r_Rm, r_t], [r_p])
            c.op("dve", lambda e: e.tensor_tensor(out=rt1[0:rot, 0:w], in0=t[0:rot, b0:b0 + w], in1=Ct[0:rot, b0:b0 + w], op=ALU.mult),
                 [r_t, r_C], [r_rt1])
            c.op("dve", lambda e: e.tensor_tensor(out=rt2[0:rot, 0:w], in0=p[0:rot, 0:w], in1=St[0:rot, b0:b0 + w], op=ALU.mult),
                 [r_p, r_S], [r_rt2])
            c.op("dve", lambda e: e.tensor_tensor(out=t[0:rot, b0:b0 + w], in0=rt1[0:rot, 0:w], in1=rt2[0:rot, 0:w], op=ALU.add),
                 [r_rt1, r_rt2], [r_t])

    lt, r_lt = c.sb("lt", [128, 256]); l2, r_l2 = c.sb("l2", [128, 2]); lam, r_lamb = c.sb("lam", [128, 1])
    c.op("dve", lambda e: e.tensor_tensor(out=lt[:, 0:128], in0=lam_s[:, 0:128], in1=lam_s[:, 128:256], op=ALU.mult), [r_lam], [r_lt])
    c.op("dve", lambda e: e.tensor_tensor(out=lt[:, 128:256], in0=lam_s[:, 256:384], in1=lam_s[:, 384:512], op=ALU.mult), [r_lam], [r_lt])
    c.op("dve", lambda e: e.tensor_reduce(out=l2[:], in_=lt[:].rearrange("p (a b) -> p a b", a=2), axis=AX.X, op=ALU.add), [r_lt], [r_l2])
    c.op("act", lambda e: e.activation(out=l2[:], in_=l2[:], func=AF.Exp), [r_l2], [r_l2])
    c.op("dve", lambda e: e.tensor_tensor(out=lam[:], in0=l2[:, 0:1], in1=l2[:, 1:2], op=ALU.subtract), [r_l2], [r_lamb])
    c.op("dve", lambda e: e.tensor_scalar(out=lam[:], in0=lam[:], scalar1=float(LAMBDA_INIT), scalar2=None, op0=ALU.add), [r_lamb], [r_lamb])

    kt = [c.sb(f"kt{i}", [128, S]) for i in range(2)]
    ki_s, r_ki = kt[0][0][0:64, :], kt[0][1]
    c.dma("sp", ki_s, kiT, [r_in], [r_ki])
    rope(ki_s, r_ki, S, TKI, 16, R16, r_R16)
    selT = [c.sb(f"selT{j}", [128, lim[j], QB]) for j in range(2)]
    big, r_big = c.sb("big", [128, QS * max(HA * 128, HB * 256, 2 * S // QS)])
    qi_s, r_qi = c.sb("qi_s", [64, HI, 128]); wi_s, r_wi = c.sb("wi_s", [128, HI])
    acc, r_acc = big[:, 0:S], r_big; wk, r_wk = big[:, S:2 * S], r_big; cb_s, r_cb = wk, r_big
    tmp, r_tmp = c.sb("tmp", [128, SB]); m8, r_m8 = c.sb("m8", [128, 8]); thr, r_thr = c.sb("thr", [128, 1])
    for qt in range(NQ // 128):
        step, qs = divmod(qt, QS)
        nst = lim[step]
        ns = nst * 128
        c.dma("sp", qi_s[:], qiT[:, :, qt * 128:(qt + 1) * 128].rearrange("h d q -> d h q"), [r_in], [r_qi])
        c.dma("sp", wi_s[:], wi[qt], [r_in], [r_wi])
        c.dma("sp", cb_s[:, 0:ns], cb[qt][:, 0:ns], [r_in], [r_cb])
        for h in range(HI):
            Ct, r_C, St, r_S = TQI
            p, r_p = ps_misc[state["m"] % 2]; state["m"] += 1
            mm(c, p[0:16, 0:128], R16[:, :], qi_s[0:16, h, :], True, True, [r_R16, r_qi], [r_p])
            c.op("dve", lambda e: e.tensor_tensor(out=rt1[0:16, 0:128], in0=qi_s[0:16, h, :], in1=Ct[0:16, qt * 128:(qt + 1) * 128], op=ALU.mult), [r_qi, r_C], [r_rt1])
            c.op("dve", lambda e: e.tensor_tensor(out=rt2[0:16, 0:128], in0=p[0:16, 0:128], in1=St[0:16, qt * 128:(qt + 1) * 128], op=ALU.mult), [r_p, r_S], [r_rt2])
            c.op("dve", lambda e: e.tensor_tensor(out=qi_s[0:16, h, :], in0=rt1[0:16, 0:128], in1=rt2[0:16, 0:128], op=ALU.add), [r_rt1, r_rt2], [r_qi])
        for h in range(HI):
            for b0 in range(0, ns, SB):
                w = min(SB, ns - b0)
                p, r_p = ps_misc[state["m"] % 2]; state["m"] += 1
                mm(c, p[:, 0:w], qi_s[:, h, :], ki_s[:, b0:b0 + w], True, True, [r_qi, r_ki], [r_p])
                if h == 0:
                    c.op("dve", lambda e: e.tensor_scalar(out=acc[:, b0:b0 + w], in0=p[:, 0:w], scalar1=0.0, scalar2=wi_s[:, h:h + 1],
                                                          op0=ALU.max, op1=ALU.mult), [r_p, r_wi], [r_acc])
                else:
                    c.op("dve", lambda e: e.tensor_scalar(out=tmp[:, 0:w], in0=p[:, 0:w], scalar1=0.0, scalar2=wi_s[:, h:h + 1],
                                                          op0=ALU.max, op1=ALU.mult), [r_p, r_wi], [r_tmp])
                    c.op("pool", lambda e: e.tensor_tensor(out=acc[:, b0:b0 + w], in0=acc[:, b0:b0 + w], in1=tmp[:, 0:w], op=ALU.add),
                         [r_acc, r_tmp], [r_acc])
        c.op("pool", lambda e: e.tensor_tensor(out=acc[:, 0:ns], in0=acc[:, 0:ns], in1=cb_s[:, 0:ns], op=ALU.add), [r_acc, r_cb], [r_acc])
        cur = acc
        r_cur = r_acc
        for it in range(TOPK // 8):
            c.op("dve", lambda e: e.max(out=m8[:], in_=cur[:, 0:ns]), [r_cur], [r_m8])
            if it < TOPK // 8 - 1:
                c.op("dve", lambda e: e.match_replace(out=wk[:, 0:ns], in_to_replace=m8[:], in_values=cur[:, 0:ns], imm_value=-3.0e38),
                     [r_m8, r_cur], [r_wk])
                cur, r_cur = wk, r_wk
        c.op("dve", lambda e: e.tensor_scalar(out=thr[:], in0=m8[:, 7:8], scalar1=-1.0e29, scalar2=None, op0=ALU.max), [r_m8], [r_thr])
        c.op("dve", lambda e: e.tensor_scalar(out=wk[:, 0:ns], in0=acc[:, 0:ns], scalar1=thr[:, 0:1], scalar2=None, op0=ALU.is_ge),
             [r_acc, r_thr], [r_wk])
        sT, r_sT = selT[step]
        for st in range(nst):
            p, r_p = ps_misc[state["m"] % 2]; state["m"] += 1
            c.op("pe", lambda e: e.transpose(p[:, 0:128], wk[:, st * 128:(st + 1) * 128], I128[:]), [r_wk, r_I], [r_p])
            c.op("act", lambda e: e.activation(out=sT[:, st, qs * 128:(qs + 1) * 128], in_=p[:, 0:128], func=AF.Copy), [r_p], [r_sT])

    TK = tables(posk, S, 0, Ck_, r_Ck, Sk_, r_Sk)
    TQ = tables(posq, NQ, 0, Cq_, r_Cq, Sq_, r_Sq)
    Pt = [c.sb(f"P{i}", [128, QB]) for i in range(2)]
    pcount = {"i": 0}

    def attend(qt_, r_q, kt_, r_k, maskT, r_mask, vt_, r_v, W, nst, scale):
        for st in range(nst):
            i = pcount["i"]; pcount["i"] += 1
            p, r_p = ps_st[i % 2]
            mm(c, p[:, 0:QB], kt_[:, st * 128:(st + 1) * 128], qt_[:, :], True, True, [r_k, r_q], [r_p])
            P, r_P = Pt[i % 2]
            c.op("act", lambda e: e.activation(out=P[:], in_=p[:, 0:QB], func=AF.Exp, scale=float(scale)), [r_p], [r_P])
            c.op("dve" if i % 2 == 0 else "pool", lambda e: e.tensor_tensor(out=P[:], in0=P[:], in1=maskT[:, st, :], op=ALU.mult), [r_P, r_mask], [r_P])
            for qs in range(QS):
                o, r_o = ps_o[qs]
                mm(c, o[:, 0:W], P[:, qs * 128:(qs + 1) * 128], vt_[:, st, 0:W], st == 0, st == nst - 1, [r_P, r_v], [r_o])

    qt2 = [c.sb(f"qt{i}", [128, QB]) for i in range(2)]
    vt = [c.sb("vt0", [128, ST, 257])] * 2
    OW = max(HA * 128, HB * 256)
    osb, r_osb = big[:, 0:QS * OW].rearrange("p (a b) -> p a b", a=QS), r_big
    rec, r_rec = c.sb("rec", [128, 1]); o2, r_o2 = c.sb("o2", [128, 256])
    sqj, r_sqj = c.sb("sqj", [128, 256]); ss, r_ss = c.sb("ss", [128, 1])
    cm_s, r_cm = selT[1]
    scale = 128 ** -0.5
    kc = {"i": 0}
    for step in range(2):
        nst = lim[step]
        sT, r_sT = selT[step]
        for kv in range(HKV):
            i = kc["i"]; kc["i"] += 1
            k_, r_k = kt[i % 2]; v_, r_v = vt[i % 2]
            c.dma("sp", k_[:], kaT[kv], [r_in], [r_k])
            rope(k_, r_k, nst * 128, TK, 32, R32, r_R32)
            c.dma("sp", v_[:, :, 0:128], va[kv].rearrange("p (t d) -> p t d", d=128), [r_in], [r_v])
            c.op("pool", lambda e: e.memset(v_[:, :, 128:129], 1.0), [], [r_v])
            for g in range(GRP):
                h = kv * GRP + g
                q_, r_q = qt2[h % 2]
                c.dma("sp", q_[:], qaT[h][:, step * QB:(step + 1) * QB], [r_in], [r_q])
                Ct, r_C, St, r_S = TQ
                tq = (Ct[:, step * QB:(step + 1) * QB], r_C, St[:, step * QB:(step + 1) * QB], r_S)
                rope(q_, r_q, QB, tq, 32, R32, r_R32)
                attend(q_, r_q, k_, r_k, sT, r_sT, v_, r_v, 129, nst, scale)
                for qs in range(QS):
                    o, r_o = ps_o[qs]
                    c.op("dve", lambda e: e.reciprocal(out=rec[:], in_=o[:, 128:129]), [r_o], [r_rec])
                    c.op("dve", lambda e: e.tensor_scalar(out=osb[:, qs, h * 128:(h + 1) * 128], in0=o[:, 0:128], scalar1=rec[:, 0:1],
                                                          scalar2=None, op0=ALU.mult), [r_o, r_rec], [r_osb])
        for qs in range(QS):
            c.dma("pool", oa[step * QB + qs * 128: step * QB + (qs + 1) * 128, :], osb[:, qs, 0:HA * 128], [r_osb], [r_oa])
    for step in range(2):
        nst = lim[step]
        c.dma("sp", cm_s[:, 0:nst, :], cmT[step].rearrange("p (t q) -> p t q", q=QB)[:, 0:nst, :], [r_in], [r_cm])
        for hd in range(HB):
            i = kc["i"]; kc["i"] += 1
            v_, r_v = vt[i % 2]
            c.dma("sp", v_[:, :, 0:256], vb[hd].rearrange("p (t d) -> p t d", d=256), [r_in], [r_v])
            c.op("pool", lambda e: e.memset(v_[:, :, 256:257], 1.0), [], [r_v])
            for j in range(2):
                h = 2 * hd + j
                k_, r_k = kt[h % 2]
                c.dma("sp", k_[:], kbT[h], [r_in], [r_k])
                rope(k_, r_k, nst * 128, TK, 32, R32, r_R32)
                q_, r_q = qt2[h % 2]
                c.dma("sp", q_[:], qbT[h][:, step * QB:(step + 1) * QB], [r_in], [r_q])
                Ct, r_C, St, r_S = TQ
                tq = (Ct[:, step * QB:(step + 1) * QB], r_C, St[:, step * QB:(step + 1) * QB], r_S)
                rope(q_, r_q, QB, tq, 32, R32, r_R32)
                attend(q_, r_q, k_, r_k, cm_s, r_cm, v_, r_v, 257, nst, scale)
                for qs in range(QS):
                    o, r_o = ps_o[qs]
                    c.op("dve", lambda e: e.reciprocal(out=rec[:], in_=o[:, 256:257]), [r_o], [r_rec])
                    if j == 0:
                        c.op("dve", lambda e: e.tensor_scalar(out=osb[:, qs, hd * 256:(hd + 1) * 256], in0=o[:, 0:256], scalar1=rec[:, 0:1],
                                                              scalar2=None, op0=ALU.mult), [r_o, r_rec], [r_osb])
                    else:
                        c.op("dve", lambda e: e.tensor_tensor(out=rec[:], in0=rec[:], in1=lam[:], op=ALU.mult), [r_rec, r_lamb], [r_rec])
                        c.op("dve", lambda e: e.scalar_tensor_tensor(out=o2[:], in0=o[:, 0:256], scalar=rec[:, 0:1],
                                                                     in1=osb[:, qs, hd * 256:(hd + 1) * 256], op0=ALU.mult, op1=ALU.subtract),
                             [r_o, r_rec, r_osb], [r_o2])
                        c.op("act", lambda e: e.activation(out=sqj[:], in_=o2[:], func=AF.Square, accum_out=ss[:]), [r_o2], [r_sqj, r_ss])
                        c.op("dve", lambda e: e.tensor_scalar(out=ss[:], in0=ss[:], scalar1=1.0 / 256, scalar2=EPS, op0=ALU.mult, op1=ALU.add), [r_ss], [r_ss])
                        c.op("act", lambda e: e.activation(out=ss[:], in_=ss[:], func=AF.Sqrt), [r_ss], [r_ss])
                        c.op("dve", lambda e: e.reciprocal(out=ss[:], in_=ss[:]), [r_ss], [r_ss])
                        c.op("dve", lambda e: e.tensor_scalar(out=o2[:], in0=o2[:], scalar1=ss[:, 0:1], scalar2=-(1.0 - LAMBDA_INIT),
                                                              op0=ALU.mult, op1=ALU.mult), [r_o2, r_ss], [r_o2])
                        c.op("pool", lambda e: e.tensor_tensor(out=osb[:, qs, hd * 256:(hd + 1) * 256], in0=o2[:], in1=gs_s[:], op=ALU.mult),
                             [r_o2, r_gs], [r_osb])
        for qs in range(QS):
            c.dma("pool", ob[step * QB + qs * 128: step * QB + (qs + 1) * 128, :], osb[:, qs, 0:HB * 256], [r_osb], [r_ob])
    c.finish([r_oa, r_ob])
    return c


def s2_consts(S, QB, hf):
    NQ = 2 * QB
    qidx = np.concatenate([np.arange(hf * QB, (hf + 1) * QB), np.arange((3 - hf) * QB, (4 - hf) * QB)])
    causal = (np.arange(S)[None, :] <= qidx[:, None])
    cb = np.where(causal, 0.0, -1.0e30).astype(np.float32).reshape(NQ // 128, 128, S)
    cm = causal.astype(np.float32)
    cmT = np.stack([np.ascontiguousarray(cm[j * QB:(j + 1) * QB].T.reshape(S // 128, 128, QB).transpose(1, 0, 2)).reshape(128, -1)
                    for j in range(2)])
    def rmat(half):
        R = np.zeros((2 * half, 2 * half), np.float32)
        for i in range(half):
            R[i, half + i] = -1.0
            R[half + i, i] = 1.0
        return np.ascontiguousarray(R.T)
    inv = np.zeros((32, 2), np.float32)
    f32 = 1.0 / (np.float32(500000.0) ** (np.arange(0, 32, 2, dtype=np.float32) / np.float32(32)))
    f16 = 1.0 / (np.float32(500000.0) ** (np.arange(0, 16, 2, dtype=np.float32) / np.float32(16)))
    inv[:, 0] = np.concatenate([f32, f32])
    inv[:16, 1] = np.concatenate([f16, f16])
    return dict(cb=cb, cmT=cmT, r32=rmat(16), r16=rmat(8), idn=np.eye(128, dtype=np.float32), inv=inv), qidx


def s2_map(pT, pos, hf, S, QB, HA, HKV, HB, HI, lam4, gsub):
    cst, qidx = s2_consts(S, QB, hf)
    ST = S // 128
    o = 0
    def take(n):
        nonlocal o
        r = pT[o:o + n]
        o += n
        return r
    qa = take(HA * 128); ka = take(HKV * 128); va = take(HKV * 128); qi = take(HI * 64); ki = take(64); wi = take(HI)
    qb = take(2 * HB * 128); kb = take(2 * HB * 128); vb = take(HB * 256)
    m = dict(cst)
    m["qaT"] = np.ascontiguousarray(qa[:, qidx]).reshape(HA, 128, -1)
    m["kaT"] = np.ascontiguousarray(ka).reshape(HKV, 128, S)
    m["va"] = np.ascontiguousarray(va.reshape(HKV, 128, ST, 128).transpose(0, 3, 2, 1)).reshape(HKV, 128, ST * 128)
    m["qiT"] = np.ascontiguousarray(qi[:, qidx]).reshape(HI, 64, -1)
    m["kiT"] = np.ascontiguousarray(ki)
    m["wi"] = np.ascontiguousarray(wi[:, qidx].T).reshape(-1, 128, HI)
    m["qbT"] = np.ascontiguousarray(qb[:, qidx]).reshape(2 * HB, 128, -1)
    m["kbT"] = np.ascontiguousarray(kb).reshape(2 * HB, 128, S)
    m["vb"] = np.ascontiguousarray(vb.reshape(HB, 256, ST, 128).transpose(0, 3, 2, 1)).reshape(HB, 128, ST * 256)
    m["posk"] = np.ascontiguousarray(np.broadcast_to(pos[None, :], (32, S))).astype(np.int32)
    m["posq"] = np.ascontiguousarray(np.broadcast_to(pos[qidx][None, :], (32, len(qidx)))).astype(np.int32)
    m["lamv"] = np.ascontiguousarray(np.broadcast_to(np.concatenate(lam4)[None, :], (128, 512)))
    m["gsub"] = np.ascontiguousarray(np.broadcast_to(gsub[None, :], (128, 256)))
    return m, qidx


def build_s3(D, FA, NT, NG, NE, TBLK=256):
    KC = D // 128
    FC = FA // 128
    NTB = NT // TBLK
    NR = NG + NG * NE
    c = Ctx()
    IN = lambda n, s, dt=F32: c.dram(n, s, dt, kind="ExternalInput")
    oaT, r_in = IN("oaT", [NTB, 128, FC * TBLK]); obT, _ = IN("obT", [NTB, 128, FC * TBLK])
    gaT, _ = IN("gaT", [KC, 128, NT]); gbT, _ = IN("gbT", [KC, 128, NT]); xT, _ = IN("xT", [KC, 128, NT])
    wa, _ = IN("wa", [KC, 128, FC * 128]); wb, _ = IN("wb", [KC, 128, FC * 128]); wo, _ = IN("wo", [KC, 128, KC * 128])
    vecs, _ = IN("vecs", [128, 4 * KC])
    wr, _ = IN("wr", [128, KC * NR]); rb, _ = IN("rb", [128, NR]); iot, _ = IN("iot", [128, 8])
    x1o, r_x1o = c.dram("x1o", [KC, 128, NT], kind="ExternalOutput")
    h2o, r_h2o = c.dram("h2o", [KC, 128, NT], kind="ExternalOutput")
    rto, r_rto = c.dram("rto", [NT, 4], kind="ExternalOutput")

    def load(name, shape, src):
        t, r = c.sb(name, shape)
        c.dma("sp", t[:], src, [r_in], [r])
        return t, r
    V, r_V = load("V", [128, 4 * KC], vecs)
    WR, r_WR = load("WR", [128, KC * NR], wr); RB, r_RB = load("RB", [128, NR], rb); IO, r_IO = load("IO", [128, 8], iot)
    ones, r_ones = c.sb("ones", [128, 128])
    c.op("dve", lambda e: e.memset(ones[:], 1.0), [], [r_ones])
    A2, r_A2 = c.sb("A2", [128, KC])
    c.op("dve", lambda e: e.tensor_scalar(out=A2[:], in0=V[:, 2 * KC:3 * KC], scalar1=1.0, scalar2=None, op0=ALU.add), [r_V], [r_A2])
    c.op("dve", lambda e: e.tensor_tensor(out=A2[:], in0=A2[:], in1=V[:, KC:2 * KC], op=ALU.mult), [r_A2, r_V], [r_A2])
    oa_s, r_oa = c.sb("oa_s", [128, FC, TBLK]); ob_s, r_ob = c.sb("ob_s", [128, FC, TBLK])
    mg, r_mg = c.sb("mg", [128, KC, TBLK]); x1, r_x1 = c.sb("x1", [128, KC, TBLK])
    was = [c.sb(f"was{i}", [128, FC, 128]) for i in range(2)]; wbs = [c.sb(f"wbs{i}", [128, FC, 128]) for i in range(2)]
    wos = [c.sb(f"wos{i}", [128, KC, 128]) for i in range(2)]
    gt = [c.sb(f"gt{i}", [128, 2, TBLK]) for i in range(2)]; xs = [c.sb(f"xs{i}", [128, TBLK]) for i in range(2)]
    t1, r_t1 = c.sb("t1", [128, TBLK]); t2, r_t2 = c.sb("t2", [128, TBLK]); sq = [c.sb(f"sq{i}", [128, TBLK]) for i in range(2)]
    rstd, r_rstd = c.sb("rstd", [128, TBLK])
    psa = [c.ps(f"psa{i}", [128, 512]) for i in range(2)]; psb = [c.ps(f"psb{i}", [128, 512]) for i in range(2)]
    pss, r_pss = c.ps("pss", [128, 512]); psr, r_psr = c.ps("psr", [128, 512])
    L, r_L = c.sb("L", [128, NR]); sm, r_sm = c.sb("sm", [128, 16]); oh, r_oh = c.sb("oh", [128, 8]); es, r_es = c.sb("es", [128, 8])
    m8, r_m8 = c.sb("m8", [128, 8]); ro, r_ro = c.sb("ro", [128, 4])
    for tb in range(NTB):
        ts = slice(tb * TBLK, (tb + 1) * TBLK)
        c.dma("sp", oa_s[:].rearrange("p a b -> p (a b)"), oaT[tb], [r_in], [r_oa])
        c.dma("sp", ob_s[:].rearrange("p a b -> p (a b)"), obT[tb], [r_in], [r_ob])
        for m in range(KC):
            wa_, r_wa = was[m % 2]; wb_, r_wb = wbs[m % 2]; g_, r_g = gt[m % 2]
            c.dma("sp", wa_[:].rearrange("p a b -> p (a b)"), wa[m], [r_in], [r_wa])
            c.dma("sp", wb_[:].rearrange("p a b -> p (a b)"), wb[m], [r_in], [r_wb])
            c.dma("sp", g_[:, 0, :], gaT[m][:, ts], [r_in], [r_g])
            c.dma("sp", g_[:, 1, :], gbT[m][:, ts], [r_in], [r_g])
            pa, r_pa = psa[m % 2]; pb, r_pb = psb[m % 2]
            for f in range(FC):
                mm(c, pa[:, 0:TBLK], wa_[:, f, :], oa_s[:, f, :], f == 0, f == FC - 1, [r_wa, r_oa], [r_pa])
            for f in range(FC):
                mm(c, pb[:, 0:TBLK], wb_[:, f, :], ob_s[:, f, :], f == 0, f == FC - 1, [r_wb, r_ob], [r_pb])
            c.op("act", lambda e: e.activation(out=g_[:], in_=g_[:], func=AF.Sigmoid), [r_g], [r_g])
            c.op("dve", lambda e: e.tensor_tensor(out=t1[:], in0=pa[:, 0:TBLK], in1=g_[:, 0, :], op=ALU.mult), [r_pa, r_g], [r_t1])
            c.op("dve", lambda e: e.tensor_tensor(out=t2[:], in0=pb[:, 0:TBLK], in1=g_[:, 1, :], op=ALU.mult), [r_pb, r_g], [r_t2])
            c.op("pool", lambda e: e.tensor_tensor(out=mg[:, m, :], in0=t1[:], in1=t2[:], op=ALU.add), [r_t1, r_t2], [r_mg])
        for m in range(KC):
            wo_, r_wo = wos[m % 2]; x_, r_x = xs[m % 2]
            c.dma("sp", wo_[:].rearrange("p a b -> p (a b)"), wo[m], [r_in], [r_wo])
            c.dma("sp", x_[:], xT[m][:, ts], [r_in], [r_x])
            pa, r_pa = psa[m % 2]
            for k in range(KC):
                mm(c, pa[:, 0:TBLK], wo_[:, k, :], mg[:, k, :], k == 0, k == KC - 1, [r_wo, r_mg], [r_pa])
            c.op("dve", lambda e: e.scalar_tensor_tensor(out=x1[:, m, :], in0=pa[:, 0:TBLK], scalar=V[:, m:m + 1], in1=x_[:],
                                                         op0=ALU.mult, op1=ALU.add), [r_pa, r_V, r_x], [r_x1])
            c.dma("pool", x1o[m][:, ts], x1[:, m, :], [r_x1], [r_x1o])
        for k in range(KC):
            s_, r_s = sq[k % 2]
            c.op("act", lambda e: e.activation(out=s_[:], in_=x1[:, k, :], func=AF.Square), [r_x1], [r_s])
            mm(c, pss[:, 0:TBLK], ones[:], s_[:], k == 0, k == KC - 1, [r_ones, r_s], [r_pss])
        c.op("dve", lambda e: e.tensor_scalar(out=rstd[:], in0=pss[:, 0:TBLK], scalar1=1.0 / D, scalar2=EPS, op0=ALU.mult, op1=ALU.add), [r_pss], [r_rstd])
        c.op("act", lambda e: e.activation(out=rstd[:], in_=rstd[:], func=AF.Sqrt), [r_rstd], [r_rstd])
        c.op("dve", lambda e: e.reciprocal(out=rstd[:], in_=rstd[:]), [r_rstd], [r_rstd])
        for k in range(KC):
            c.op("dve", lambda e: e.scalar_tensor_tensor(out=mg[:, k, :], in0=x1[:, k, :], scalar=A2[:, k:k + 1], in1=rstd[:],
                                                         op0=ALU.mult, op1=ALU.mult), [r_x1, r_A2, r_rstd], [r_mg])
            c.op("pool", lambda e: e.tensor_scalar(out=mg[:, k, :], in0=mg[:, k, :], scalar1=V[:, 3 * KC + k:3 * KC + k + 1], scalar2=None,
                                                   op0=ALU.add), [r_mg, r_V], [r_mg])
            c.dma("pool", h2o[k][:, ts], mg[:, k, :], [r_mg], [r_h2o])
        for sub in range(TBLK // 128):
            for k in range(KC):
                mm(c, psr[:, 0:NR], mg[:, k, sub * 128:(sub + 1) * 128], WR[:, k * NR:(k + 1) * NR], k == 0, k == KC - 1, [r_mg, r_WR], [r_psr])
            c.op("dve", lambda e: e.tensor_tensor(out=L[:], in0=psr[:, 0:NR], in1=RB[:], op=ALU.add), [r_psr, r_RB], [r_L])
            D_ = lambda fn, rd, wrt: c.op("dve", fn, rd, wrt)
            D_(lambda e: e.tensor_reduce(out=sm[:, 0:1], in_=L[:, 0:NG], axis=AX.X, op=ALU.max), [r_L], [r_sm])
            D_(lambda e: e.tensor_scalar(out=sm[:, 1:2], in0=sm[:, 0:1], scalar1=-1.0, scalar2=None, op0=ALU.mult), [r_sm], [r_sm])
            c.op("act", lambda e: e.activation(out=es[:, 0:NG], in_=L[:, 0:NG], func=AF.Exp, bias=sm[:, 1:2], scale=1.0), [r_L, r_sm], [r_es])
            D_(lambda e: e.tensor_reduce(out=sm[:, 2:3], in_=es[:, 0:NG], axis=AX.X, op=ALU.add), [r_es], [r_sm])
            D_(lambda e: e.reciprocal(out=sm[:, 3:4], in_=sm[:, 2:3]), [r_sm], [r_sm])
            D_(lambda e: e.tensor_scalar(out=oh[:, 0:NG], in0=L[:, 0:NG], scalar1=sm[:, 0:1], scalar2=None, op0=ALU.is_equal), [r_L, r_sm], [r_oh])
            D_(lambda e: e.tensor_tensor(out=es[:, 0:NG], in0=oh[:, 0:NG], in1=IO[:, 0:NG], op=ALU.mult), [r_oh, r_IO], [r_es])
            D_(lambda e: e.tensor_reduce(out=sm[:, 4:5], in_=es[:, 0:NG], axis=AX.X, op=ALU.add), [r_es], [r_sm])
            for g in range(NG):
                seg = L[:, NG + g * NE: NG + (g + 1) * NE]
                if g == 0:
                    D_(lambda e: e.tensor_scalar(out=es[:, 0:NE], in0=seg, scalar1=oh[:, 0:1], scalar2=None, op0=ALU.mult), [r_L, r_oh], [r_es])
                else:
                    D_(lambda e: e.scalar_tensor_tensor(out=es[:, 0:NE], in0=seg, scalar=oh[:, g:g + 1], in1=es[:, 0:NE], op0=ALU.mult, op1=ALU.add),
                       [r_L, r_oh, r_es], [r_es])
            D_(lambda e: e.max(out=m8[:], in_=es[:, 0:NE]), [r_es], [r_m8])
            D_(lambda e: e.tensor_tensor(out=sm[:, 7:8], in0=m8[:, 1:2], in1=m8[:, 0:1], op=ALU.subtract), [r_m8], [r_sm])
            c.op("act", lambda e: e.activation(out=sm[:, 7:8], in_=sm[:, 7:8], func=AF.Exp), [r_sm], [r_sm])
            D_(lambda e: e.tensor_scalar(out=sm[:, 8:9], in0=sm[:, 7:8], scalar1=1.0, scalar2=None, op0=ALU.add), [r_sm], [r_sm])
            D_(lambda e: e.reciprocal(out=sm[:, 9:10], in_=sm[:, 8:9]), [r_sm], [r_sm])
            D_(lambda e: e.tensor_tensor(out=sm[:, 10:11], in0=sm[:, 7:8], in1=sm[:, 9:10], op=ALU.mult), [r_sm], [r_sm])
            D_(lambda e: e.tensor_tensor(out=ro[:, 2:3], in0=sm[:, 9:10], in1=sm[:, 3:4], op=ALU.mult), [r_sm], [r_ro])
            D_(lambda e: e.tensor_tensor(out=ro[:, 3:4], in0=sm[:, 10:11], in1=sm[:, 3:4], op=ALU.mult), [r_sm], [r_ro])
            for j in range(2):
                D_(lambda e: e.tensor_scalar(out=oh[:, 0:NE], in0=es[:, 0:NE], scalar1=m8[:, j:j + 1], scalar2=None, op0=ALU.is_equal), [r_es, r_m8], [r_oh])
                D_(lambda e: e.tensor_tensor(out=oh[:, 0:NE], in0=oh[:, 0:NE], in1=IO[:, 0:NE], op=ALU.mult), [r_oh, r_IO], [r_oh])
                D_(lambda e: e.tensor_reduce(out=sm[:, 11 + j:12 + j], in_=oh[:, 0:NE], axis=AX.X, op=ALU.add), [r_oh], [r_sm])
                D_(lambda e: e.scalar_tensor_tensor(out=ro[:, j:j + 1], in0=sm[:, 4:5], scalar=float(NE), in1=sm[:, 11 + j:12 + j], op0=ALU.mult, op1=ALU.add),
                   [r_sm], [r_ro])
            c.dma("pool", rto[tb * TBLK + sub * 128: tb * TBLK + (sub + 1) * 128, :], ro[:], [r_ro], [r_rto])
    c.finish([r_x1o, r_h2o, r_rto])
    return c


def build_s4(D, FE, NEL, C):
    KC = D // 128
    FT = FE // 128
    c = Ctx()
    IN = lambda n, s, dt=F32: c.dram(n, s, dt, kind="ExternalInput")
    xg, r_in = IN("xg", [NEL, KC, 128, C]); ws, _ = IN("ws", [NEL, 128, C])
    wg, _ = IN("wg", [NEL * FT, 128, KC * 128]); wu, _ = IN("wu", [NEL * FT, 128, KC * 128]); wd, _ = IN("wd", [NEL * KC, 128, FT * 128])
    yo, r_yo = c.dram("yo", [NEL, KC, 128, C], kind="ExternalOutput")
    blocks = [(b0, min(512, C - b0)) for b0 in range(0, C, 512)]
    xs, r_xs = c.sb("xs", [128, KC, 512]); act, r_act = c.sb("act", [128, FT, 512]); wsl, r_wsl = c.sb("wsl", [128, 512])
    wgs = [c.sb(f"wg{i}", [128, KC, 128]) for i in range(2)]; wus = [c.sb(f"wu{i}", [128, KC, 128]) for i in range(2)]
    wds = [c.sb(f"wd{i}", [128, FT, 128]) for i in range(2)]
    sg, r_sg = c.sb("sg", [128, 512]); yt = [c.sb(f"yt{i}", [128, 512]) for i in range(2)]
    psg = [c.ps(f"psg{i}", [128, 512]) for i in range(2)]; psu = [c.ps(f"psu{i}", [128, 512]) for i in range(2)]
    psy = [c.ps(f"psy{i}", [128, 512]) for i in range(2)]
    n = 0
    for e_ in range(NEL):
        for (b0, w) in blocks:
            c.dma("sp", xs[:, :, 0:w], xg[e_].rearrange("k p c -> p k c")[:, :, b0:b0 + w], [r_in], [r_xs])
            c.dma("sp", wsl[:, 0:w], ws[e_][:, b0:b0 + w], [r_in], [r_wsl])
            for f in range(FT):
                g_, r_g = wgs[n % 2]; u_, r_u = wus[n % 2]
                c.dma("sp", g_[:].rearrange("p a b -> p (a b)"), wg[e_ * FT + f], [r_in], [r_g])
                c.dma("sp", u_[:].rearrange("p a b -> p (a b)"), wu[e_ * FT + f], [r_in], [r_u])
                pg, r_pg = psg[n % 2]; pu, r_pu = psu[n % 2]
                for k in range(KC):
                    mm(c, pg[:, 0:w], g_[:, k, :], xs[:, k, 0:w], k == 0, k == KC - 1, [r_g, r_xs], [r_pg])
                for k in range(KC):
                    mm(c, pu[:, 0:w], u_[:, k, :], xs[:, k, 0:w], k == 0, k == KC - 1, [r_u, r_xs], [r_pu])
                c.op("act", lambda e: e.activation(out=sg[:, 0:w], in_=pg[:, 0:w], func=AF.Silu), [r_pg], [r_sg])
                c.op("dve", lambda e: e.tensor_tensor(out=act[:, f, 0:w], in0=pu[:, 0:w], in1=sg[:, 0:w], op=ALU.mult), [r_pu, r_sg], [r_act])
                n += 1
            for m in range(KC):
                d_, r_d = wds[m % 2]
                c.dma("sp", d_[:].rearrange("p a b -> p (a b)"), wd[e_ * KC + m], [r_in], [r_d])
                py, r_py = psy[m % 2]
                for f in range(FT):
                    mm(c, py[:, 0:w], d_[:, f, :], act[:, f, 0:w], f == 0, f == FT - 1, [r_d, r_act], [r_py])
                y_, r_y = yt[m % 2]
                c.op("dve", lambda e: e.tensor_tensor(out=y_[:, 0:w], in0=py[:, 0:w], in1=wsl[:, 0:w], op=ALU.mult), [r_py, r_wsl], [r_y])
                c.dma("pool", yo[e_, m][:, b0:b0 + w], y_[:, 0:w], [r_y], [r_yo])
    c.finish([r_yo])
    return c


def build_s5(D, NT, TBLK=512):
    KC = D // 128
    NTB = NT // TBLK
    c = Ctx()
    IN = lambda n, s, dt=F32: c.dram(n, s, dt, kind="ExternalInput")
    x1, r_in = IN("x1", [KC, 128, NT]); y1, _ = IN("y1", [KC, 128, NT]); y2, _ = IN("y2", [KC, 128, NT]); vecs, _ = IN("vecs", [128, 2 * KC])
    oo, r_oo = c.dram("oo", [KC, 128, NT], kind="ExternalOutput")
    V, r_V = c.sb("V", [128, 2 * KC])
    c.dma("sp", V[:], vecs, [r_in], [r_V])
    ones, r_ones = c.sb("ones", [128, 128])
    c.op("dve", lambda e: e.memset(ones[:], 1.0), [], [r_ones])
    z, r_z = c.sb("z", [128, KC, TBLK]); rstd, r_rstd = c.sb("rstd", [128, TBLK])
    ld = [[c.sb(f"ld{i}_{j}", [128, TBLK]) for j in range(3)] for i in range(2)]
    sq = [c.sb(f"sq{i}", [128, TBLK]) for i in range(2)]; ot = [c.sb(f"ot{i}", [128, TBLK]) for i in range(2)]
    pss, r_pss = c.ps("pss", [128, 512])
    for tb in range(NTB):
        ts = slice(tb * TBLK, (tb + 1) * TBLK)
        for m in range(KC):
            (a, r_a), (b, r_b), (x, r_x) = ld[m % 2]
            c.dma("sp", a[:], y1[m][:, ts], [r_in], [r_a]); c.dma("sp", b[:], y2[m][:, ts], [r_in], [r_b]); c.dma("sp", x[:], x1[m][:, ts], [r_in], [r_x])
            c.op("pool", lambda e: e.tensor_tensor(out=a[:], in0=a[:], in1=b[:], op=ALU.add), [r_a, r_b], [r_a])
            c.op("dve", lambda e: e.scalar_tensor_tensor(out=z[:, m, :], in0=a[:], scalar=V[:, m:m + 1], in1=x[:], op0=ALU.mult, op1=ALU.add),
                 [r_a, r_V, r_x], [r_z])
            s_, r_s = sq[m % 2]
            c.op("act", lambda e: e.activation(out=s_[:], in_=z[:, m, :], func=AF.Square), [r_z], [r_s])
            mm(c, pss[:, 0:TBLK], ones[:], s_[:], m == 0, m == KC - 1, [r_ones, r_s], [r_pss])
        c.op("dve", lambda e: e.tensor_scalar(out=rstd[:], in0=pss[:, 0:TBLK], scalar1=1.0 / D, scalar2=EPS, op0=ALU.mult, op1=ALU.add), [r_pss], [r_rstd])
        c.op("act", lambda e: e.activation(out=rstd[:], in_=rstd[:], func=AF.Sqrt), [r_rstd], [r_rstd])
        c.op("dve", lambda e: e.reciprocal(out=rstd[:], in_=rstd[:]), [r_rstd], [r_rstd])
        for m in range(KC):
            o_, r_o = ot[m % 2]
            c.op("dve", lambda e: e.scalar_tensor_tensor(out=o_[:], in0=z[:, m, :], scalar=V[:, KC + m:KC + m + 1], in1=rstd[:], op0=ALU.mult, op1=ALU.mult),
                 [r_z, r_V, r_rstd], [r_o])
            c.dma("pool", oo[m][:, ts], o_[:], [r_o], [r_oo])
    c.finish([r_oo])
    return c


def kernel(x, c, positions, w_ada, b_ada, g_norm1, w_in, lam_q1, lam_k1, lam_q2, lam_k2, g_subln,
           w_proj_a, w_proj_b, w_out, g_norm2, router_g, router_g_b, router_e, router_e_b,
           w_gate, w_up, w_down, g_final):
    f = lambda a: np.asarray(a, dtype=np.float32)
    x = f(x); B, S, D = x.shape
    KC = D // 128
    T = B * S
    NT = T // NCORES
    HA, HKV, HB, HI = 16, 4, 8, 16
    NG, NE = 4, 8
    FE = w_gate.shape[-1]
    x2d = x.reshape(T, D)
    mod = run_s0(f(c), f(w_ada)[0], f(b_ada)[0])
    shift1, scale1, gate1, shift2, scale2, gate2 = np.split(mod, 6, axis=1)
    projT = run_s1(x2d, f(w_in)[0], f(g_norm1)[0], scale1, shift1, TPB=S // 512)
    QB = S // 4
    ctx = build_s2(S, QB, HA, HKV, HB, HI, min(256, S // 4), 0.2)
    pos = np.asarray(positions).astype(np.int32)
    lam4 = [f(lam_q1)[0], f(lam_k1)[0], f(lam_q2)[0], f(lam_k2)[0]]
    maps, qidxs = [], []
    for r in range(NCORES):
        b, hf = divmod(r, 2)
        m, qidx = s2_map(projT[:, b * S:(b + 1) * S], pos[b], hf, S, QB, HA, HKV, HB, HI, lam4, f(g_subln)[0])
        maps.append(m); qidxs.append(qidx)
    res = ctx.run(maps)
    o_a = np.zeros((T, HA * 128), np.float32); o_b = np.zeros((T, HB * 256), np.float32)
    for r in range(NCORES):
        b = r // 2
        o_a[b * S + qidxs[r]] = res[r]["oa"]; o_b[b * S + qidxs[r]] = res[r]["ob"]
    del maps, res
    off_g = HA * 128 + 2 * HKV * 128 + HI * 64 + 64 + HI + 4 * HB * 128 + HB * 256
    NR = NG + NG * NE
    wr_full = np.concatenate([f(router_g)[0], f(router_e)[0].transpose(1, 0, 2).reshape(D, NG * NE)], axis=1)
    rbias = np.concatenate([f(router_g_b)[0], f(router_e_b)[0].reshape(-1)])
    wa_t = tile_w(f(w_proj_a)[0], (HA * 128) // 128); wb_t = tile_w(f(w_proj_b)[0], (HB * 256) // 128); wo_t = tile_w(f(w_out)[0], KC)
    ctx = build_s3(D, HA * 128, NT, NG, NE)
    maps = []
    for r in range(NCORES):
        b = r // 2
        ts = slice(r * NT, (r + 1) * NT)
        maps.append({
            "oaT": tile_xT(o_a[ts], 256), "obT": tile_xT(o_b[ts], 256),
            "gaT": np.ascontiguousarray(projT[off_g:off_g + D, ts]).reshape(KC, 128, NT),
            "gbT": np.ascontiguousarray(projT[off_g + D:off_g + 2 * D, ts]).reshape(KC, 128, NT),
            "xT": np.ascontiguousarray(x2d[ts].T).reshape(KC, 128, NT),
            "wa": wa_t, "wb": wb_t, "wo": wo_t,
            "vecs": np.concatenate([vec_pk(gate1[b], KC), vec_pk(f(g_norm2)[0], KC), vec_pk(scale2[b], KC), vec_pk(shift2[b], KC)], axis=1),
            "wr": np.ascontiguousarray(wr_full.reshape(KC, 128, NR).transpose(1, 0, 2)).reshape(128, KC * NR),
            "rb": np.ascontiguousarray(np.broadcast_to(rbias[None, :], (128, NR))),
            "iot": np.ascontiguousarray(np.broadcast_to(np.arange(8, dtype=np.float32)[None, :], (128, 8))),
        })
    res = ctx.run(maps)
    x1T = [res[r]["x1o"] for r in range(NCORES)]
    h2 = np.concatenate([res[r]["h2o"].reshape(D, NT).T for r in range(NCORES)], axis=0)
    route = np.concatenate([res[r]["rto"] for r in range(NCORES)], axis=0)
    del maps, res, projT
    eid = np.rint(route[:, 0:2]).astype(np.int64)
    NEXP = NG * NE
    NEL = NEXP // NCORES
    lists = [[np.nonzero(eid[:, k] == e)[0] for k in range(2)] for e in range(NEXP)]
    cmax = max(len(l[0]) + len(l[1]) for l in lists)
    C = max(128, -(-cmax // 32) * 32)
    ctx = build_s4(D, FE, NEL, C)
    maps = []
    for r in range(NCORES):
        xg = np.zeros((NEL, KC, 128, C), np.float32); ws = np.zeros((NEL, 128, C), np.float32)
        wg_l, wu_l, wd_l = [], [], []
        for el in range(NEL):
            e = r * NEL + el
            toks = np.concatenate(lists[e]); n = len(toks)
            wsl = np.concatenate([route[lists[e][0], 2], route[lists[e][1], 3]])
            xg[el, :, :, :n] = h2[toks].T.reshape(KC, 128, n)
            ws[el, :, :n] = wsl[None, :]
            wg_l.append(tile_w(f(w_gate[0, e]), KC)); wu_l.append(tile_w(f(w_up[0, e]), KC)); wd_l.append(tile_w(f(w_down[0, e]), FE // 128))
        maps.append({"xg": xg, "ws": ws, "wg": np.concatenate(wg_l), "wu": np.concatenate(wu_l), "wd": np.concatenate(wd_l)})
    res = ctx.run(maps)
    ys = [np.zeros((T, D), np.float32), np.zeros((T, D), np.float32)]
    for r in range(NCORES):
        yo = res[r]["yo"]
        for el in range(NEL):
            e = r * NEL + el
            yT = yo[el].reshape(D, C)
            n0 = len(lists[e][0]); n1 = len(lists[e][1])
            ys[0][lists[e][0]] = yT[:, :n0].T
            ys[1][lists[e][1]] = yT[:, n0:n0 + n1].T
    del maps, res, h2
    ctx = build_s5(D, NT)
    maps = []
    for r in range(NCORES):
        b = r // 2
        ts = slice(r * NT, (r + 1) * NT)
        maps.append({"x1": x1T[r], "y1": np.ascontiguousarray(ys[0][ts].T).reshape(KC, 128, NT),
                     "y2": np.ascontiguousarray(ys[1][ts].T).reshape(KC, 128, NT),
                     "vecs": np.concatenate([vec_pk(gate2[b], KC), vec_pk(f(g_final), KC)], axis=1)})
    res = ctx.run(maps)
    out = np.concatenate([res[r]["oo"].reshape(D, NT).T for r in range(NCORES)], axis=0)
    return np.ascontiguousarray(out.reshape(B, S, D)).astype(np.float32)
```

```python
import numpy as np
from contextlib import ExitStack
import concourse.bass as bass
import concourse.mybir as mybir
from concourse.bass_utils import run_bass_kernel_spmd

F32 = mybir.dt.float32
I32 = mybir.dt.int32
AF = mybir.ActivationFunctionType
ALU = mybir.AluOpType
AX = mybir.AxisListType
NCORES = 8
EPS = 1e-6


class Res:
    __slots__ = ("name", "lw", "rd")

    def __init__(self, name):
        self.name = name
        self.lw = None
        self.rd = {}


class Ctx:
    def __init__(self):
        self.nc = bass.Bass("TRN2", target_bir_lowering=False)
        self.es = ExitStack()
        nc = self.nc
        self.eng = {"pe": nc.tensor, "act": nc.scalar, "dve": nc.vector, "pool": nc.gpsimd, "sp": nc.sync}
        self.sems = {}
        self.cnt = {}
        self.waited = {k: {} for k in self.eng}
        for k in ("pe", "act", "dve", "pool"):
            self._sem("E_" + k)
        self.psum_i = 0

    def _sem(self, key):
        if key not in self.sems:
            self.sems[key] = self.es.enter_context(self.nc.semaphore("s_" + key))
            self.cnt[key] = 0
        return self.sems[key]

    def dram(self, name, shape, dt=F32, kind="Internal"):
        t = self.nc.dram_tensor(name, list(shape), dt, kind=kind)
        return t.ap(), Res(name)

    def sb(self, name, shape, dt=F32):
        t = self.es.enter_context(self.nc.sbuf_tensor(name, list(shape), dt))
        return t, Res(name)

    def ps(self, name, shape, dt=F32):
        t = self.es.enter_context(self.nc.psum_tensor(name, list(shape), dt))
        return t, Res(name)

    def _wait(self, en, deps, keep_last=False):
        w = self.waited[en]
        e = self.eng[en]
        need = {}
        for key, val in deps:
            if en == "pe" and key == "E_pe":
                continue
            if w.get(key, 0) < val and need.get(key, 0) < val:
                need[key] = val
        items = list(need.items())
        last = items.pop() if (keep_last and items) else None
        for key, val in items:
            e.wait_ge(self.sems[key], val)
            w[key] = val
        if last is not None:
            w[last[0]] = last[1]
        return last

    def _deps(self, reads, writes):
        deps = []
        for r in reads:
            if r.lw is not None:
                deps.append(r.lw)
        for wr in writes:
            if wr.lw is not None:
                deps.append(wr.lw)
            deps.extend(wr.rd.items())
        return deps

    def _mark(self, tok, reads, writes):
        for r in reads:
            if r.rd.get(tok[0], 0) < tok[1]:
                r.rd[tok[0]] = tok[1]
        for wr in writes:
            wr.lw = tok
            wr.rd = {}

    def op(self, en, fn, reads=(), writes=()):
        last = self._wait(en, self._deps(reads, writes), keep_last=True)
        ins = fn(self.eng[en])
        if last is not None:
            ins = ins._wait_ge(self.sems[last[0]], last[1])
        key = "E_" + en
        self.cnt[key] += 1
        ins.then_inc(self.sems[key], 1)
        self._mark((key, self.cnt[key]), reads, writes)

    def dma(self, en, out, in_, reads, writes, **kw):
        last = self._wait(en, self._deps(reads, writes), keep_last=True)
        key = "D_" + writes[0].name
        self._sem(key)
        ins = self.eng[en].dma_start(out=out, in_=in_, **kw)
        if last is not None:
            ins = ins._wait_ge(self.sems[last[0]], last[1])
        self.cnt[key] += 16
        ins.then_inc(self.sems[key], 16)
        self._mark((key, self.cnt[key]), reads, writes)

    def finish(self, outs):
        deps = []
        for r in outs:
            if r.lw is not None:
                deps.append(r.lw)
        self._wait("sp", deps)

    def run(self, in_maps):
        self.es.close()
        res = run_bass_kernel_spmd(self.nc, in_maps, core_ids=list(range(NCORES)))
        return res.results


def mm(ctx, out, lhsT, rhs, start, stop, reads, writes):
    ctx.op("pe", lambda e: e.matmul(out, lhsT, rhs, start=start, stop=stop), reads, writes)


def build_s0(D, NB, NCOL):
    KC = D // 128
    NBLK = NCOL // 512
    c = Ctx()
    cT, r_cT = c.dram("cT", [128, KC * NB], kind="ExternalInput")
    w, r_w = c.dram("w", [NBLK, 128, KC * 512], kind="ExternalInput")
    b, r_b = c.dram("b", [NB, NCOL], kind="ExternalInput")
    o, r_o = c.dram("o", [NB, NCOL], kind="ExternalOutput")
    cs, r_cs = c.sb("cs", [128, KC * NB])
    bs, r_bs = c.sb("bs", [NB, NCOL])
    os_, r_os = c.sb("os", [NB, NCOL])
    wt = [c.sb(f"wt{i}", [128, KC * 512]) for i in range(2)]
    pst = [c.ps(f"ps{i}", [128, 512]) for i in range(2)]
    c.dma("sp", cs[:], cT, [r_cT], [r_cs])
    c.dma("sp", bs[:], b, [r_b], [r_bs])
    c.op("act", lambda e: e.activation(out=cs[:], in_=cs[:], func=AF.Silu), [r_cs], [r_cs])
    for n in range(NBLK):
        wtile, r_wt = wt[n % 2]
        c.dma("sp", wtile[:], w[n], [r_w], [r_wt])
        p, r_p = pst[n % 2]
        for k in range(KC):
            mm(c, p[0:NB, :], cs[:, k * NB:(k + 1) * NB], wtile[:, k * 512:(k + 1) * 512],
               k == 0, k == KC - 1, [r_cs, r_wt], [r_p])
        c.op("dve", lambda e: e.tensor_tensor(out=os_[:, n * 512:(n + 1) * 512], in0=p[0:NB, :],
                                              in1=bs[:, n * 512:(n + 1) * 512], op=ALU.add),
             [r_p, r_bs], [r_os])
    c.dma("sp", o, os_[:], [r_os], [r_o])
    c.finish([r_o])
    return c


def run_s0(c_in, w_ada, b_ada):
    NB, D = c_in.shape
    NTOT = w_ada.shape[1]
    NCOL = NTOT // NCORES
    KC = D // 128
    ctx = build_s0(D, NB, NCOL)
    cT = np.ascontiguousarray(c_in.T.reshape(KC, 128, NB).transpose(1, 0, 2).reshape(128, KC * NB))
    maps = []
    for r in range(NCORES):
        ws = w_ada[:, r * NCOL:(r + 1) * NCOL]
        wt = ws.reshape(KC, 128, NCOL // 512, 512).transpose(2, 1, 0, 3)
        maps.append({"cT": cT, "w": np.ascontiguousarray(wt).reshape(NCOL // 512, 128, KC * 512),
                     "b": np.ascontiguousarray(np.broadcast_to(b_ada[None, r * NCOL:(r + 1) * NCOL], (NB, NCOL)))})
    res = ctx.run(maps)
    return np.concatenate([res[r]["o"] for r in range(NCORES)], axis=1)


def build_s1(D, TB, MT, TBLK=512, TPB=4):
    KC = D // 128
    NBATCH = TB // TPB
    c = Ctx()
    xT, r_xT = c.dram("xT", [TB, 128, KC * TBLK], kind="ExternalInput")
    w, r_w = c.dram("w", [MT, 128, KC * 128], kind="ExternalInput")
    g, r_g = c.dram("g", [128, KC], kind="ExternalInput")
    sc, r_sc = c.dram("sc", [128, KC * NBATCH], kind="ExternalInput")
    sh, r_sh = c.dram("sh", [128, KC * NBATCH], kind="ExternalInput")
    o, r_o = c.dram("o", [MT * 128, TB * TBLK], kind="ExternalOutput")
    gs, r_gs = c.sb("gs", [128, KC])
    A, r_A = c.sb("A", [128, KC, NBATCH])
    S, r_S = c.sb("S", [128, KC, NBATCH])
    ones, r_ones = c.sb("ones", [128, 128])
    xt = [c.sb(f"xt{i}", [128, KC, TBLK]) for i in range(2)]
    wt = [c.sb(f"wt{i}", [128, KC, 128]) for i in range(2)]
    sq = [c.sb(f"sq{i}", [128, TBLK]) for i in range(2)]
    ot = [c.sb(f"ot{i}", [128, TBLK]) for i in range(2)]
    rstd, r_rstd = c.sb("rstd", [128, TBLK])
    pss, r_pss = c.ps("pss", [128, TBLK])
    psm = [c.ps(f"psm{i}", [128, TBLK]) for i in range(2)]
    c.dma("sp", gs[:], g, [r_g], [r_gs])
    c.dma("sp", A[:].rearrange("p k b -> p (k b)"), sc, [r_sc], [r_A])
    c.dma("sp", S[:].rearrange("p k b -> p (k b)"), sh, [r_sh], [r_S])
    c.op("dve", lambda e: e.memset(ones[:], 1.0), [], [r_ones])
    c.op("dve", lambda e: e.tensor_scalar(out=A[:], in0=A[:], scalar1=1.0, scalar2=None, op0=ALU.add), [r_A], [r_A])
    for bb in range(NBATCH):
        c.op("dve", lambda e: e.tensor_tensor(out=A[:, :, bb], in0=A[:, :, bb], in1=gs[:], op=ALU.mult),
             [r_A, r_gs], [r_A])
    def load_x(tb):
        x, r_x = xt[tb % 2]
        c.dma("sp", x[:].rearrange("p k t -> p (k t)"), xT[tb], [r_xT], [r_x])

    def prep(tb):
        bb = tb // TPB
        x, r_x = xt[tb % 2]
        for k in range(KC):
            s, r_s = sq[k % 2]
            c.op("act", lambda e: e.activation(out=s[:], in_=x[:, k, :], func=AF.Square), [r_x], [r_s])
            mm(c, pss[:], ones[:], s[:], k == 0, k == KC - 1, [r_ones, r_s], [r_pss])
        c.op("dve", lambda e: e.tensor_scalar(out=rstd[:], in0=pss[:], scalar1=1.0 / D, scalar2=EPS,
                                              op0=ALU.mult, op1=ALU.add), [r_pss], [r_rstd])
        c.op("act", lambda e: e.activation(out=rstd[:], in_=rstd[:], func=AF.Sqrt), [r_rstd], [r_rstd])
        c.op("dve", lambda e: e.reciprocal(out=rstd[:], in_=rstd[:]), [r_rstd], [r_rstd])
        for k in range(KC):
            c.op("dve", lambda e: e.scalar_tensor_tensor(out=x[:, k, :], in0=x[:, k, :], scalar=A[:, k, bb:bb + 1],
                                                         in1=rstd[:], op0=ALU.mult, op1=ALU.mult),
                 [r_x, r_A, r_rstd], [r_x])
            c.op("pool", lambda e: e.tensor_scalar(out=x[:, k, :], in0=x[:, k, :], scalar1=S[:, k, bb:bb + 1],
                                                   scalar2=None, op0=ALU.add), [r_x, r_S], [r_x])

    wi = 0
    load_x(0)
    prep(0)
    for tb in range(TB):
        x, r_x = xt[tb % 2]
        if tb + 1 < TB:
            load_x(tb + 1)
        for m in range(MT):
            if m == min(3, MT - 1) and tb + 1 < TB:
                prep(tb + 1)
            wtile, r_wt = wt[wi % 2]
            c.dma("sp", wtile[:].rearrange("p k n -> p (k n)"), w[m], [r_w], [r_wt])
            p, r_p = psm[wi % 2]
            for k in range(KC):
                mm(c, p[:], wtile[:, k, :], x[:, k, :], k == 0, k == KC - 1, [r_wt, r_x], [r_p])
            oo, r_oo = ot[wi % 2]
            c.op("act", lambda e: e.activation(out=oo[:], in_=p[:], func=AF.Copy), [r_p], [r_oo])
            c.dma("pool", o[m * 128:(m + 1) * 128, tb * TBLK:(tb + 1) * TBLK], oo[:], [r_oo], [r_o])
            wi += 1
    c.finish([r_o])
    return c


def tile_w(wm, KC):
    K, N = wm.shape
    return np.ascontiguousarray(wm.reshape(KC, 128, N // 128, 128).transpose(2, 1, 0, 3)).reshape(N // 128, 128, KC * 128)


def tile_xT(xm, TBLK):
    T, D = xm.shape
    KC = D // 128
    return np.ascontiguousarray(xm.reshape(T // TBLK, TBLK, KC, 128).transpose(0, 3, 2, 1)).reshape(T // TBLK, 128, KC * TBLK)


def vec_pk(v, KC):
    v = np.asarray(v)
    lead = v.shape[:-1]
    vv = v.reshape(-1, KC, 128).transpose(2, 1, 0)
    return np.ascontiguousarray(vv).reshape(128, -1) if lead else np.ascontiguousarray(vv[:, :, 0])


def run_s1(x2d, w_in, g1, scale1, shift1, TPB):
    T, D = x2d.shape
    KC = D // 128
    N = w_in.shape[1]
    TBLK = 512
    NTH, NCG = 2, NCORES // 2
    TBH = T // TBLK // NTH
    MT = -(-N // (128 * NCG))
    NP = MT * 128 * NCG
    NBH = scale1.shape[0] // NTH
    wp = np.zeros((D, NP), np.float32)
    wp[:, :N] = w_in
    ctx = build_s1(D, TBH, MT, TBLK, TPB)
    xT = tile_xT(x2d, TBLK)
    wts = [tile_w(wp[:, cg * MT * 128:(cg + 1) * MT * 128], KC) for cg in range(NCG)]
    gk = vec_pk(g1, KC)
    maps = []
    for r in range(NCORES):
        th, cg = divmod(r, NCG)
        maps.append({"xT": xT[th * TBH:(th + 1) * TBH], "w": wts[cg], "g": gk,
                     "sc": vec_pk(scale1[th * NBH:(th + 1) * NBH], KC), "sh": vec_pk(shift1[th * NBH:(th + 1) * NBH], KC)})
    res = ctx.run(maps)
    out = np.empty((NP, T), np.float32)
    TH = T // NTH
    for r in range(NCORES):
        th, cg = divmod(r, NCG)
        out[cg * MT * 128:(cg + 1) * MT * 128, th * TH:(th + 1) * TH] = res[r]["o"]
    return out[:N]


def build_s2(S, QB, HA, HKV, HB, HI, TOPK, LAMBDA_INIT):
    ST = S // 128
    QS = QB // 128
    NQ = 2 * QB
    GRP = HA // HKV
    SB = min(512, S)
    NSB = S // SB
    lim = [2 * QS, 4 * QS]
    c = Ctx()
    IN = lambda n, s, dt=F32: c.dram(n, s, dt, kind="ExternalInput")
    qaT, r_in = IN("qaT", [HA, 128, NQ]); kaT, _ = IN("kaT", [HKV, 128, S]); va, _ = IN("va", [HKV, 128, ST * 128])
    qiT, _ = IN("qiT", [HI, 64, NQ]); kiT, _ = IN("kiT", [64, S]); wi, _ = IN("wi", [NQ // 128, 128, HI])
    qbT, _ = IN("qbT", [2 * HB, 128, NQ]); kbT, _ = IN("kbT", [2 * HB, 128, S]); vb, _ = IN("vb", [HB, 128, ST * 256])
    posk, _ = IN("posk", [32, S], I32); posq, _ = IN("posq", [32, NQ], I32)
    inv, _ = IN("inv", [32, 2]); cb, _ = IN("cb", [NQ // 128, 128, S]); cmT, _ = IN("cmT", [2, 128, ST * QB])
    lamv, _ = IN("lamv", [128, 4 * 128]); gsub, _ = IN("gsub", [128, 256])
    r32, _ = IN("r32", [32, 32]); r16, _ = IN("r16", [16, 16]); idn, _ = IN("idn", [128, 128])
    oa, r_oa = c.dram("oa", [NQ, HA * 128], kind="ExternalOutput")
    ob, r_ob = c.dram("ob", [NQ, HB * 256], kind="ExternalOutput")

    def load(name, shape, src, dt=F32, eng="sp"):
        t, r = c.sb(name, shape, dt)
        c.dma(eng, t[:] if len(shape) == 2 else t[:].rearrange("p a b -> p (a b)"), src, [r_in], [r])
        return t, r

    inv_s, r_inv = load("inv_s", [32, 2], inv)
    R32, r_R32 = load("R32", [32, 32], r32); R16, r_R16 = load("R16", [16, 16], r16); I128, r_I = load("I128", [128, 128], idn)
    lam_s, r_lam = load("lam_s", [128, 512], lamv); gs_s, r_gs = load("gs_s", [128, 256], gsub)
    PI = float(np.pi)

    npi, r_npi = c.sb("npi", [32, 1])
    c.op("dve", lambda e: e.memset(npi[:], -PI), [], [r_npi])

    pi_, r_pi = c.sb("tb_pi", [32, 512], I32)
    pf, r_pf = c.sb("tb_pf", [32, 512])
    rr, r_rr = c.sb("tb_rr", [32, 512])
    Ck_, r_Ck = c.sb("tb_Ck", [32, S]); Sk_, r_Sk = c.sb("tb_Sk", [32, S])
    Cq_, r_Cq = c.sb("tb_Cq", [32, NQ]); Sq_, r_Sq = c.sb("tb_Sq", [32, NQ])

    def tables(pos_ap, n, col, Ct, r_C, St, r_S):
        for b0 in range(0, n, 512):
            w = min(512, n - b0)
            c.dma("sp", pi_[:, 0:w], pos_ap[:, b0:b0 + w], [r_in], [r_pi])
            c.op("dve", lambda e: e.tensor_copy(out=pf[:, 0:w], in_=pi_[:, 0:w]), [r_pi], [r_pf])
            c.op("dve", lambda e: e.tensor_scalar(out=pf[:, 0:w], in0=pf[:, 0:w], scalar1=inv_s[:, col:col + 1], scalar2=None,
                                                  op0=ALU.mult), [r_pf, r_inv], [r_pf])
            for dst, r_d, off in ((St, r_S, 0.0), (Ct, r_C, PI / 2)):
                c.op("dve", lambda e: e.tensor_scalar(out=rr[:, 0:w], in0=pf[:, 0:w], scalar1=off, scalar2=1.0 / (2 * PI),
                                                      op0=ALU.add, op1=ALU.mult), [r_pf], [r_rr])
                c.op("dve", lambda e: e.tensor_copy(out=pi_[:, 0:w], in_=rr[:, 0:w]), [r_rr], [r_pi])
                c.op("dve", lambda e: e.tensor_copy(out=rr[:, 0:w], in_=pi_[:, 0:w]), [r_pi], [r_rr])
                c.op("dve", lambda e: e.scalar_tensor_tensor(out=dst[:, b0:b0 + w], in0=rr[:, 0:w], scalar=-2 * PI, in1=pf[:, 0:w],
                                                             op0=ALU.mult, op1=ALU.add), [r_rr, r_pf], [r_d])
                if off != 0.0:
                    c.op("dve", lambda e: e.tensor_scalar(out=dst[:, b0:b0 + w], in0=dst[:, b0:b0 + w], scalar1=off, scalar2=None,
                                                          op0=ALU.add), [r_d], [r_d])
                c.op("dve", lambda e: e.tensor_scalar(out=rr[:, 0:w], in0=dst[:, b0:b0 + w], scalar1=PI, scalar2=None,
                                                      op0=ALU.is_gt), [r_d], [r_rr])
                c.op("dve", lambda e: e.scalar_tensor_tensor(out=dst[:, b0:b0 + w], in0=rr[:, 0:w], scalar=-2 * PI, in1=dst[:, b0:b0 + w],
                                                             op0=ALU.mult, op1=ALU.add), [r_rr, r_d], [r_d])
                c.op("act", lambda e: e.activation(out=dst[:, b0:b0 + w], in_=dst[:, b0:b0 + w], func=AF.Sin), [r_d], [r_d])
        return (Ct, r_C, St, r_S)

    TKI = tables(posk, S, 1, Ck_, r_Ck, Sk_, r_Sk)
    TQI = tables(posq, NQ, 1, Cq_, r_Cq, Sq_, r_Sq)

    ps_misc = [c.ps(f"psm{i}", [128, 512]) for i in range(2)]
    ps_st = [c.ps(f"pst{i}", [128, 512]) for i in range(2)]
    ps_o = [c.ps(f"pso{i}", [128, 512]) for i in range(4)]
    rt1, r_rt1 = c.sb("rt1", [32, 512]); rt2, r_rt2 = c.sb("rt2", [32, 512])
    state = {"m": 0}

    def rope(t, r_t, n, tab, rot, Rm, r_Rm):
        Ct, r_C, St, r_S = tab
        for b0 in range(0, n, 512):
            w = min(512, n - b0)
            p, r_p = ps_misc[state["m"] % 2]; state["m"] += 1
            mm(c, p[0:rot, 0:w], Rm[:, :], t[0:rot, b0:b0 + w], True, True, [r_Rm, r_t], [r_p])
            c.op("dve", lambda e: e.tensor_tensor(out=rt1[0:rot, 0:w], in0=t[0:rot, b0:b0 + w], in1=Ct[0:rot, b0:b0 + w], op=ALU.mult),
                 [r_t, r_C], [r_rt1])
            c.op("dve", lambda e: e.tensor_tensor(out=rt2[0:rot, 0:w], in0=p[0:rot, 0:w], in1=St[0:rot, b0:b0 + w], op=ALU.mult),
                 [r_p, r_S], [r_rt2])
            c.op("dve", lambda e: e.tensor_tensor(out=t[0:rot, b0:b0 + w], in0=rt1[0:rot, 0:w], in1=rt2[0:rot, 0:w], op=ALU.add),
                 [r_rt1, r_rt2], [r_t])

    lt, r_lt = c.sb("lt", [128, 256]); l2, r_l2 = c.sb("l2", [128, 2]); lam, r_lamb = c.sb("lam", [128, 1])
    c.op("dve", lambda e: e.tensor_tensor(out=lt[:, 0:128], in0=lam_s[:, 0:128], in1=lam_s[:, 128:256], op=ALU.mult), [r_lam], [r_lt])
    c.op("dve", lambda e: e.tensor_tensor(out=lt[:, 128:256], in0=lam_s[:, 256:384], in1=lam_s[:, 384:512], op=ALU.mult), [r_lam], [r_lt])
    c.op("dve", lambda e: e.tensor_reduce(out=l2[:], in_=lt[:].rearrange("p (a b) -> p a b", a=2), axis=AX.X, op=ALU.add), [r_lt], [r_l2])
    c.op("act", lambda e: e.activation(out=l2[:], in_=l2[:], func=AF.Exp), [r_l2], [r_l2])
    c.op("dve", lambda e: e.tensor_tensor(out=lam[:], in0=l2[:, 0:1], in1=l2[:, 1:2], op=ALU.subtract), [r_l2], [r_lamb])
    c.op("dve", lambda e: e.tensor_scalar(out=lam[:], in0=lam[:], scalar1=float(LAMBDA_INIT), scalar2=None, op0=ALU.add), [r_lamb], [r_lamb])

    kt = [c.sb(f"kt{i}", [128, S]) for i in range(2)]
    ki_s, r_ki = kt[0][0][0:64, :], kt[0][1]
    c.dma("sp", ki_s, kiT, [r_in], [r_ki])
    rope(ki_s, r_ki, S, TKI, 16, R16, r_R16)
    selT = [c.sb(f"selT{j}", [128, lim[j], QB]) for j in range(2)]
    big, r_big = c.sb("big", [128, QS * max(HA * 128, HB * 256, 2 * S // QS)])
    qi_s, r_qi = c.sb("qi_s", [64, HI, 128]); wi_s, r_wi = c.sb("wi_s", [128, HI])
    acc, r_acc = big[:, 0:S], r_big; wk, r_wk = big[:, S:2 * S], r_big; cb_s, r_cb = wk, r_big
    tmp, r_tmp = c.sb("tmp", [128, SB]); m8, r_m8 = c.sb("m8", [128, 8]); thr, r_thr = c.sb("thr", [128, 1])
    for qt in range(NQ // 128):
        step, qs = divmod(qt, QS)
        nst = lim[step]
        ns = nst * 128
        c.dma("sp", qi_s[:], qiT[:, :, qt * 128:(qt + 1) * 128].rearrange("h d q -> d h q"), [r_in], [r_qi])
        c.dma("sp", wi_s[:], wi[qt], [r_in], [r_wi])
        c.dma("sp", cb_s[:, 0:ns], cb[qt][:, 0:ns], [r_in], [r_cb])
        for h in range(HI):
            Ct, r_C, St, r_S = TQI
            p, r_p = ps_misc[state["m"] % 2]; state["m"] += 1
            mm(c, p[0:16, 0:128], R16[:, :], qi_s[0:16, h, :], True, True, [r_R16, r_qi], [r_p])
            c.op("dve", lambda e: e.tensor_tensor(out=rt1[0:16, 0:128], in0=qi_s[0:16, h, :], in1=Ct[0:16, qt * 128:(qt + 1) * 128], op=ALU.mult), [r_qi, r_C], [r_rt1])
            c.op("dve", lambda e: e.tensor_tensor(out=rt2[0:16, 0:128], in0=p[0:16, 0:128], in1=St[0:16, qt * 128:(qt + 1) * 128], op=ALU.mult), [r_p, r_S], [r_rt2])
            c.op("dve", lambda e: e.tensor_tensor(out=qi_s[0:16, h, :], in0=rt1[0:16, 0:128], in1=rt2[0:16, 0:128], op=ALU.add), [r_rt1, r_rt2], [r_qi])
        for h in range(HI):
            for b0 in range(0, ns, SB):
                w = min(SB, ns - b0)
                p, r_p = ps_misc[state["m"] % 2]; state["m"] += 1
                mm(c, p[:, 0:w], qi_s[:, h, :], ki_s[:, b0:b0 + w], True, True, [r_qi, r_ki], [r_p])
                if h == 0:
                    c.op("dve", lambda e: e.tensor_scalar(out=acc[:, b0:b0 + w], in0=p[:, 0:w], scalar1=0.0, scalar2=wi_s[:, h:h + 1],
                                                          op0=ALU.max, op1=ALU.mult), [r_p, r_wi], [r_acc])
                else:
                    c.op("dve", lambda e: e.tensor_scalar(out=tmp[:, 0:w], in0=p[:, 0:w], scalar1=0.0, scalar2=wi_s[:, h:h + 1],
                                                          op0=ALU.max, op1=ALU.mult), [r_p, r_wi], [r_tmp])
                    c.op("pool", lambda e: e.tensor_tensor(out=acc[:, b0:b0 + w], in0=acc[:, b0:b0 + w], in1=tmp[:, 0:w], op=ALU.add),
                         [r_acc, r_tmp], [r_acc])
        c.op("pool", lambda e: e.tensor_tensor(out=acc[:, 0:ns], in0=acc[:, 0:ns], in1=cb_s[:, 0:ns], op=ALU.add), [r_acc, r_cb], [r_acc])
        cur = acc
        r_cur = r_acc
        for it in range(TOPK // 8):
            c.op("dve", lambda e: e.max(out=m8[:], in_=cur[:, 0:ns]), [r_cur], [r_m8])
            if it < TOPK // 8 - 1:
                c.op("dve", lambda e: e.match_replace(out=wk[:, 0:ns], in_to_replace=m8[:], in_values=cur[:, 0:ns], imm_value=-3.0e38),
                     [r_m8, r_cur], [r_wk])
                cur, r_cur = wk, r_wk
        c.op("dve", lambda e: e.tensor_scalar(out=thr[:], in0=m8[:, 7:8], scalar1=-1.0e29, scalar2=None, op0=ALU.max), [r_m8], [r_thr])
        c.op("dve", lambda e: e.tensor_scalar(out=wk[:, 0:ns], in0=acc[:, 0:ns], scalar1=thr[:, 0:1], scalar2=None, op0=ALU.is_ge),
             [r_acc, r_thr], [r_wk])
        sT, r_sT = selT[step]
        for st in range(nst):
            p, r_p = ps_misc[state["m"] % 2]; state["m"] += 1
            c.op("pe", lambda e: e.transpose(p[:, 0:128], wk[:, st * 128:(st + 1) * 128], I128[:]), [r_wk, r_I], [r_p])
            c.op("act", lambda e: e.activation(out=sT[:, st, qs * 128:(qs + 1) * 128], in_=p[:, 0:128], func=AF.Copy), [r_p], [r_sT])

    TK = tables(posk, S, 0, Ck_, r_Ck, Sk_, r_Sk)
    TQ = tables(posq, NQ, 0, Cq_, r_Cq, Sq_, r_Sq)
    Pt = [c.sb(f"P{i}", [128, QB]) for i in range(2)]
    pcount = {"i": 0}

    def attend(qt_, r_q, kt_, r_k, maskT, r_mask, vt_, r_v, W, nst, scale):
        for st in range(nst):
            i = pcount["i"]; pcount["i"] += 1
            p, r_p = ps_st[i % 2]
            mm(c, p[:, 0:QB], kt_[:, st * 128:(st + 1) * 128], qt_[:, :], True, True, [r_k, r_q], [r_p])
            P, r_P = Pt[i % 2]
            c.op("act", lambda e: e.activation(out=P[:], in_=p[:, 0:QB], func=AF.Exp, scale=float(scale)), [r_p], [r_P])
            c.op("dve" if i % 2 == 0 else "pool", lambda e: e.tensor_tensor(out=P[:], in0=P[:], in1=maskT[:, st, :], op=ALU.mult), [r_P, r_mask], [r_P])
            for qs in range(QS):
                o, r_o = ps_o[qs]
                mm(c, o[:, 0:W], P[:, qs * 128:(qs + 1) * 128], vt_[:, st, 0:W], st == 0, st == nst - 1, [r_P, r_v], [r_o])

    qt2 = [c.sb(f"qt{i}", [128, QB]) for i in range(2)]
    vt = [c.sb("vt0", [128, ST, 257])] * 2
    OW = max(HA * 128, HB * 256)
    osb, r_osb = big[:, 0:QS * OW].rearrange("p (a b) -> p a b", a=QS), r_big
    rec, r_rec = c.sb("rec", [128, 1]); o2, r_o2 = c.sb("o2", [128, 256])
    sqj, r_sqj = c.sb("sqj", [128, 256]); ss, r_ss = c.sb("ss", [128, 1])
    cm_s, r_cm = selT[1]
    scale = 128 ** -0.5
    kc = {"i": 0}
    for step in range(2):
        nst = lim[step]
        sT, r_sT = selT[step]
        for kv in range(HKV):
            i = kc["i"]; kc["i"] += 1
            k_, r_k = kt[i % 2]; v_, r_v = vt[i % 2]
            c.dma("sp", k_[:], kaT[kv], [r_in], [r_k])
            rope(k_, r_k, nst * 128, TK, 32, R32, r_R32)
            c.dma("sp", v_[:, :, 0:128], va[kv].rearrange("p (t d) -> p t d", d=128), [r_in], [r_v])
            c.op("pool", lambda e: e.memset(v_[:, :, 128:129], 1.0), [], [r_v])
            for g in range(GRP):
                h = kv * GRP + g
                q_, r_q = qt2[h % 2]
                c.dma("sp", q_[:], qaT[h][:, step * QB:(step + 1) * QB], [r_in], [r_q])
                Ct, r_C, St, r_S = TQ
                tq = (Ct[:, step * QB:(step + 1) * QB], r_C, St[:, step * QB:(step + 1) * QB], r_S)
                rope(q_, r_q, QB, tq, 32, R32, r_R32)
                attend(q_, r_q, k_, r_k, sT, r_sT, v_, r_v, 129, nst, scale)
                for qs in range(QS):
                    o, r_o = ps_o[qs]
                    c.op("dve", lambda e: e.reciprocal(out=rec[:], in_=o[:, 128:129]), [r_o], [r_rec])
                    c.op("dve", lambda e: e.tensor_scalar(out=osb[:, qs, h * 128:(h + 1) * 128], in0=o[:, 0:128], scalar1=rec[:, 0:1],
                                                          scalar2=None, op0=ALU.mult), [r_o, r_rec], [r_osb])
        for qs in range(QS):
            c.dma("pool", oa[step * QB + qs * 128: step * QB + (qs + 1) * 128, :], osb[:, qs, 0:HA * 128], [r_osb], [r_oa])
    for step in range(2):
        nst = lim[step]
        c.dma("sp", cm_s[:, 0:nst, :], cmT[step].rearrange("p (t q) -> p t q", q=QB)[:, 0:nst, :], [r_in], [r_cm])
        for hd in range(HB):
            i = kc["i"]; kc["i"] += 1
            v_, r_v = vt[i % 2]
            c.dma("sp", v_[:, :, 0:256], vb[hd].rearrange("p (t d) -> p t d", d=256), [r_in], [r_v])
            c.op("pool", lambda e: e.memset(v_[:, :, 256:257], 1.0), [], [r_v])
            for j in range(2):
                h = 2 * hd + j
                k_, r_k = kt[h % 2]
                c.dma("sp", k_[:], kbT[h], [r_in], [r_k])
                rope(k_, r_k, nst * 128, TK, 32, R32, r_R32)
                q_, r_q = qt2[h % 2]
                c.dma("sp", q_[:], qbT[h][:, step * QB:(step + 1) * QB], [r_in], [r_q])
                Ct, r_C, St, r_S = TQ
                tq = (Ct[:, step * QB:(step + 1) * QB], r_C, St[:, step * QB:(step + 1) * QB], r_S)
                rope(q_, r_q, QB, tq, 32, R32, r_R32)
                attend(q_, r_q, k_, r_k, cm_s, r_cm, v_, r_v, 257, nst, scale)
                for qs in range(QS):
                    o, r_o = ps_o[qs]
                    c.op("dve", lambda e: e.reciprocal(out=rec[:], in_=o[:, 256:257]), [r_o], [r_rec])
                    if j == 0:
                        c.op("dve", lambda e: e.tensor_scalar(out=osb[:, qs, hd * 256:(hd + 1) * 256], in0=o[:, 0:256], scalar1=rec[:, 0:1],
                                                              scalar2=None, op0=ALU.mult), [r_o, r_rec], [r_osb])
                    else:
                        c.op("dve", lambda e: e.tensor_tensor(out=rec[:], in0=rec[:], in1=lam[:], op=ALU.mult), [r_rec, r_lamb], [r_rec])
                        c.op("dve", lambda e: e.scalar_tensor_tensor(out=o2[:], in0=o[:, 0:256], scalar=rec[:, 0:1],
                                                                     in1=osb[:, qs, hd * 256:(hd + 1) * 256], op0=ALU.mult, op1=ALU.subtract),
                             [r_o, r_rec, r_osb], [r_o2])
                        c.op("act", lambda e: e.activation(out=sqj[:], in_=o2[:], func=AF.Square, accum_out=ss[:]), [r_o2], [r_sqj, r_ss])
                        c.op("dve", lambda e: e.tensor_scalar(out=ss[:], in0=ss[:], scalar1=1.0 / 256, scalar2=EPS, op0=ALU.mult, op1=ALU.add), [r_ss], [r_ss])
                        c.op("act", lambda e: e.activation(out=ss[:], in_=ss[:], func=AF.Sqrt), [r_ss], [r_ss])
                        c.op("dve", lambda e: e.reciprocal(out=ss[:], in_=ss[:]), [r_ss], [r_ss])
                        c.op("dve", lambda e: e.tensor_scalar(out=o2[:], in0=o2[:], scalar1=ss[:, 0:1], scalar2=-(1.0 - LAMBDA_INIT),
                                                              op0=ALU.mult, op1=ALU.mult), [r_o2, r_ss], [r_o2])
                        c.op("pool", lambda e: e.tensor_tensor(out=osb[:, qs, hd * 256:(hd + 1) * 256], in0=o2[:], in1=gs_s[:], op=ALU.mult),
                             [r_o2, r_gs], [r_osb])
        for qs in range(QS):
            c.dma("pool", ob[step * QB + qs * 128: step * QB + (qs + 1) * 128, :], osb[:, qs, 0:HB * 256], [r_osb], [r_ob])
    c.finish([r_oa, r_ob])
    return c


def s2_consts(S, QB, hf):
    NQ = 2 * QB
    qidx = np.concatenate([np.arange(hf * QB, (hf + 1) * QB), np.arange((3 - hf) * QB, (4 - hf) * QB)])
    causal = (np.arange(S)[None, :] <= qidx[:, None])
    cb = np.where(causal, 0.0, -1.0e30).astype(np.float32).reshape(NQ // 128, 128, S)
    cm = causal.astype(np.float32)
    cmT = np.stack([np.ascontiguousarray(cm[j * QB:(j + 1) * QB].T.reshape(S // 128, 128, QB).transpose(1, 0, 2)).reshape(128, -1)
                    for j in range(2)])
    def rmat(half):
        R = np.zeros((2 * half, 2 * half), np.float32)
        for i in range(half):
            R[i, half + i] = -1.0
            R[half + i, i] = 1.0
        return np.ascontiguousarray(R.T)
    inv = np.zeros((32, 2), np.float32)
    f32 = 1.0 / (np.float32(500000.0) ** (np.arange(0, 32, 2, dtype=np.float32) / np.float32(32)))
    f16 = 1.0 / (np.float32(500000.0) ** (np.arange(0, 16, 2, dtype=np.float32) / np.float32(16)))
    inv[:, 0] = np.concatenate([f32, f32])
    inv[:16, 1] = np.concatenate([f16, f16])
    return dict(cb=cb, cmT=cmT, r32=rmat(16), r16=rmat(8), idn=np.eye(128, dtype=np.float32), inv=inv), qidx


def s2_map(pT, pos, hf, S, QB, HA, HKV, HB, HI, lam4, gsub):
    cst, qidx = s2_consts(S, QB, hf)
    ST = S // 128
    o = 0
    def take(n):
        nonlocal o
        r = pT[o:o + n]
        o += n
        return r
    qa = take(HA * 128); ka = take(HKV * 128); va = take(HKV * 128); qi = take(HI * 64); ki = take(64); wi = take(HI)
    qb = take(2 * HB * 128); kb = take(2 * HB * 128); vb = take(HB * 256)
    m = dict(cst)
    m["qaT"] = np.ascontiguousarray(qa[:, qidx]).reshape(HA, 128, -1)
    m["kaT"] = np.ascontiguousarray(ka).reshape(HKV, 128, S)
    m["va"] = np.ascontiguousarray(va.reshape(HKV, 128, ST, 128).transpose(0, 3, 2, 1)).reshape(HKV, 128, ST * 128)
    m["qiT"] = np.ascontiguousarray(qi[:, qidx]).reshape(HI, 64, -1)
    m["kiT"] = np.ascontiguousarray(ki)
    m["wi"] = np.ascontiguousarray(wi[:, qidx].T).reshape(-1, 128, HI)
    m["qbT"] = np.ascontiguousarray(qb[:, qidx]).reshape(2 * HB, 128, -1)
    m["kbT"] = np.ascontiguousarray(kb).reshape(2 * HB, 128, S)
    m["vb"] = np.ascontiguousarray(vb.reshape(HB, 256, ST, 128).transpose(0, 3, 2, 1)).reshape(HB, 128, ST * 256)
    m["posk"] = np.ascontiguousarray(np.broadcast_to(pos[None, :], (32, S))).astype(np.int32)
    m["posq"] = np.ascontiguousarray(np.broadcast_to(pos[qidx][None, :], (32, len(qidx)))).astype(np.int32)
    m["lamv"] = np.ascontiguousarray(np.broadcast_to(np.concatenate(lam4)[None, :], (128, 512)))
    m["gsub"] = np.ascontiguousarray(np.broadcast_to(gsub[None, :], (128, 256)))
    return m, qidx


def build_s3(D, FA, NT, NG, NE, TBLK=256):
    KC = D // 128
    FC = FA // 128
    NTB = NT // TBLK
    NR = NG + NG * NE
    c = Ctx()
    IN = lambda n, s, dt=F32: c.dram(n, s, dt, kind="ExternalInput")
    oaT, r_in = IN("oaT", [NTB, 128, FC * TBLK]); obT, _ = IN("obT", [NTB, 128, FC * TBLK])
    gaT, _ = IN("gaT", [KC, 128, NT]); gbT, _ = IN("gbT", [KC, 128, NT]); xT, _ = IN("xT", [KC, 128, NT])
    wa, _ = IN("wa", [KC, 128, FC * 128]); wb, _ = IN("wb", [KC, 128, FC * 128]); wo, _ = IN("wo", [KC, 128, KC * 128])
    vecs, _ = IN("vecs", [128, 4 * KC])
    wr, _ = IN("wr", [128, KC * NR]); rb, _ = IN("rb", [128, NR]); iot, _ = IN("iot", [128, 8])
    x1o, r_x1o = c.dram("x1o", [KC, 128, NT], kind="ExternalOutput")
    h2o, r_h2o = c.dram("h2o", [KC, 128, NT], kind="ExternalOutput")
    rto, r_rto = c.dram("rto", [NT, 4], kind="ExternalOutput")

    def load(name, shape, src):
        t, r = c.sb(name, shape)
        c.dma("sp", t[:], src, [r_in], [r])
        return t, r
    V, r_V = load("V", [128, 4 * KC], vecs)
    WR, r_WR = load("WR", [128, KC * NR], wr); RB, r_RB = load("RB", [128, NR], rb); IO, r_IO = load("IO", [128, 8], iot)
    ones, r_ones = c.sb("ones", [128, 128])
    c.op("dve", lambda e: e.memset(ones[:], 1.0), [], [r_ones])
    A2, r_A2 = c.sb("A2", [128, KC])
    c.op("dve", lambda e: e.tensor_scalar(out=A2[:], in0=V[:, 2 * KC:3 * KC], scalar1=1.0, scalar2=None, op0=ALU.add), [r_V], [r_A2])
    c.op("dve", lambda e: e.tensor_tensor(out=A2[:], in0=A2[:], in1=V[:, KC:2 * KC], op=ALU.mult), [r_A2, r_V], [r_A2])
    oa_s, r_oa = c.sb("oa_s", [128, FC, TBLK]); ob_s, r_ob = c.sb("ob_s", [128, FC, TBLK])
    mg, r_mg = c.sb("mg", [128, KC, TBLK]); x1, r_x1 = c.sb("x1", [128, KC, TBLK])
    was = [c.sb(f"was{i}", [128, FC, 128]) for i in range(2)]; wbs = [c.sb(f"wbs{i}", [128, FC, 128]) for i in range(2)]
    wos = [c.sb(f"wos{i}", [128, KC, 128]) for i in range(2)]
    gt = [c.sb(f"gt{i}", [128, 2, TBLK]) for i in range(2)]; xs = [c.sb(f"xs{i}", [128, TBLK]) for i in range(2)]
    t1, r_t1 = c.sb("t1", [128, TBLK]); t2, r_t2 = c.sb("t2", [128, TBLK]); sq = [c.sb(f"sq{i}", [128, TBLK]) for i in range(2)]
    rstd, r_rstd = c.sb("rstd", [128, TBLK])
    psa = [c.ps(f"psa{i}", [128, 512]) for i in range(2)]; psb = [c.ps(f"psb{i}", [128, 512]) for i in range(2)]
    pss, r_pss = c.ps("pss", [128, 512]); psr, r_psr = c.ps("psr", [128, 512])
    L, r_L = c.sb("L", [128, NR]); sm, r_sm = c.sb("sm", [128, 16]); oh, r_oh = c.sb("oh", [128, 8]); es, r_es = c.sb("es", [128, 8])
    m8, r_m8 = c.sb("m8", [128, 8]); ro, r_ro = c.sb("ro", [128, 4])
    for tb in range(NTB):
        ts = slice(tb * TBLK, (tb + 1) * TBLK)
        c.dma("sp", oa_s[:].rearrange("p a b -> p (a b)"), oaT[tb], [r_in], [r_oa])
        c.dma("sp", ob_s[:].rearrange("p a b -> p (a b)"), obT[tb], [r_in], [r_ob])
        for m in range(KC):
            wa_, r_wa = was[m % 2]; wb_, r_wb = wbs[m % 2]; g_, r_g = gt[m % 2]
            c.dma("sp", wa_[:].rearrange("p a b -> p (a b)"), wa[m], [r_in], [r_wa])
            c.dma("sp", wb_[:].rearrange("p a b -> p (a b)"), wb[m], [r_in], [r_wb])
            c.dma("sp", g_[:, 0, :], gaT[m][:, ts], [r_in], [r_g])
            c.dma("sp", g_[:, 1, :], gbT[m][:, ts], [r_in], [r_g])
            pa, r_pa = psa[m % 2]; pb, r_pb = psb[m % 2]
            for f in range(FC):
                mm(c, pa[:, 0:TBLK], wa_[:, f, :], oa_s[:, f, :], f == 0, f == FC - 1, [r_wa, r_oa], [r_pa])
            for f in range(FC):
                mm(c, pb[:, 0:TBLK], wb_[:, f, :], ob_s[:, f, :], f == 0, f == FC - 1, [r_wb, r_ob], [r_pb])
            c.op("act", lambda e: e.activation(out=g_[:], in_=g_[:], func=AF.Sigmoid), [r_g], [r_g])
            c.op("dve", lambda e: e.tensor_tensor(out=t1[:], in0=pa[:, 0:TBLK], in1=g_[:, 0, :], op=ALU.mult), [r_pa, r_g], [r_t1])
            c.op("dve", lambda e: e.tensor_tensor(out=t2[:], in0=pb[:, 0:TBLK], in1=g_[:, 1, :], op=ALU.mult), [r_pb, r_g], [r_t2])
            c.op("pool", lambda e: e.tensor_tensor(out=mg[:, m, :], in0=t1[:], in1=t2[:], op=ALU.add), [r_t1, r_t2], [r_mg])
        for m in range(KC):
            wo_, r_wo = wos[m % 2]; x_, r_x = xs[m % 2]
            c.dma("sp", wo_[:].rearrange("p a b -> p (a b)"), wo[m], [r_in], [r_wo])
            c.dma("sp", x_[:], xT[m][:, ts], [r_in], [r_x])
            pa, r_pa = psa[m % 2]
            for k in range(KC):
                mm(c, pa[:, 0:TBLK], wo_[:, k, :], mg[:, k, :], k == 0, k == KC - 1, [r_wo, r_mg], [r_pa])
            c.op("dve", lambda e: e.scalar_tensor_tensor(out=x1[:, m, :], in0=pa[:, 0:TBLK], scalar=V[:, m:m + 1], in1=x_[:],
                                                         op0=ALU.mult, op1=ALU.add), [r_pa, r_V, r_x], [r_x1])
            c.dma("pool", x1o[m][:, ts], x1[:, m, :], [r_x1], [r_x1o])
        for k in range(KC):
            s_, r_s = sq[k % 2]
            c.op("act", lambda e: e.activation(out=s_[:], in_=x1[:, k, :], func=AF.Square), [r_x1], [r_s])
            mm(c, pss[:, 0:TBLK], ones[:], s_[:], k == 0, k == KC - 1, [r_ones, r_s], [r_pss])
        c.op("dve", lambda e: e.tensor_scalar(out=rstd[:], in0=pss[:, 0:TBLK], scalar1=1.0 / D, scalar2=EPS, op0=ALU.mult, op1=ALU.add), [r_pss], [r_rstd])
        c.op("act", lambda e: e.activation(out=rstd[:], in_=rstd[:], func=AF.Sqrt), [r_rstd], [r_rstd])
        c.op("dve", lambda e: e.reciprocal(out=rstd[:], in_=rstd[:]), [r_rstd], [r_rstd])
        for k in range(KC):
            c.op("dve", lambda e: e.scalar_tensor_tensor(out=mg[:, k, :], in0=x1[:, k, :], scalar=A2[:, k:k + 1], in1=rstd[:],
                                                         op0=ALU.mult, op1=ALU.mult), [r_x1, r_A2, r_rstd], [r_mg])
            c.op("pool", lambda e: e.tensor_scalar(out=mg[:, k, :], in0=mg[:, k, :], scalar1=V[:, 3 * KC + k:3 * KC + k + 1], scalar2=None,
                                                   op0=ALU.add), [r_mg, r_V], [r_mg])
            c.dma("pool", h2o[k][:, ts], mg[:, k, :], [r_mg], [r_h2o])
        for sub in range(TBLK // 128):
            for k in range(KC):
                mm(c, psr[:, 0:NR], mg[:, k, sub * 128:(sub + 1) * 128], WR[:, k * NR:(k + 1) * NR], k == 0, k == KC - 1, [r_mg, r_WR], [r_psr])
            c.op("dve", lambda e: e.tensor_tensor(out=L[:], in0=psr[:, 0:NR], in1=RB[:], op=ALU.add), [r_psr, r_RB], [r_L])
            D_ = lambda fn, rd, wrt: c.op("dve", fn, rd, wrt)
            D_(lambda e: e.tensor_reduce(out=sm[:, 0:1], in_=L[:, 0:NG], axis=AX.X, op=ALU.max), [r_L], [r_sm])
            D_(lambda e: e.tensor_scalar(out=sm[:, 1:2], in0=sm[:, 0:1], scalar1=-1.0, scalar2=None, op0=ALU.mult), [r_sm], [r_sm])
            c.op("act", lambda e: e.activation(out=es[:, 0:NG], in_=L[:, 0:NG], func=AF.Exp, bias=sm[:, 1:2], scale=1.0), [r_L, r_sm], [r_es])
            D_(lambda e: e.tensor_reduce(out=sm[:, 2:3], in_=es[:, 0:NG], axis=AX.X, op=ALU.add), [r_es], [r_sm])
            D_(lambda e: e.reciprocal(out=sm[:, 3:4], in_=sm[:, 2:3]), [r_sm], [r_sm])
            D_(lambda e: e.tensor_scalar(out=oh[:, 0:NG], in0=L[:, 0:NG], scalar1=sm[:, 0:1], scalar2=None, op0=ALU.is_equal), [r_L, r_sm], [r_oh])
            D_(lambda e: e.tensor_tensor(out=es[:, 0:NG], in0=oh[:, 0:NG], in1=IO[:, 0:NG], op=ALU.mult), [r_oh, r_IO], [r_es])
            D_(lambda e: e.tensor_reduce(out=sm[:, 4:5], in_=es[:, 0:NG], axis=AX.X, op=ALU.add), [r_es], [r_sm])
            for g in range(NG):
                seg = L[:, NG + g * NE: NG + (g + 1) * NE]
                if g == 0:
                    D_(lambda e: e.tensor_scalar(out=es[:, 0:NE], in0=seg, scalar1=oh[:, 0:1], scalar2=None, op0=ALU.mult), [r_L, r_oh], [r_es])
                else:
                    D_(lambda e: e.scalar_tensor_tensor(out=es[:, 0:NE], in0=seg, scalar=oh[:, g:g + 1], in1=es[:, 0:NE], op0=ALU.mult, op1=ALU.add),
                       [r_L, r_oh, r_es], [r_es])
            D_(lambda e: e.max(out=m8[:], in_=es[:, 0:NE]), [r_es], [r_m8])
            D_(lambda e: e.tensor_tensor(out=sm[:, 7:8], in0=m8[:, 1:2], in1=m8[:, 0:1], op=ALU.subtract), [r_m8], [r_sm])
            c.op("act", lambda e: e.activation(out=sm[:, 7:8], in_=sm[:, 7:8], func=AF.Exp), [r_sm], [r_sm])
            D_(lambda e: e.tensor_scalar(out=sm[:, 8:9], in0=sm[:, 7:8], scalar1=1.0, scalar2=None, op0=ALU.add), [r_sm], [r_sm])
            D_(lambda e: e.reciprocal(out=sm[:, 9:10], in_=sm[:, 8:9]), [r_sm], [r_sm])
            D_(lambda e: e.tensor_tensor(out=sm[:, 10:11], in0=sm[:, 7:8], in1=sm[:, 9:10], op=ALU.mult), [r_sm], [r_sm])
            D_(lambda e: e.tensor_tensor(out=ro[:, 2:3], in0=sm[:, 9:10], in1=sm[:, 3:4], op=ALU.mult), [r_sm], [r_ro])
            D_(lambda e: e.tensor_tensor(out=ro[:, 3:4], in0=sm[:, 10:11], in1=sm[:, 3:4], op=ALU.mult), [r_sm], [r_ro])
            for j in range(2):
                D_(lambda e: e.tensor_scalar(out=oh[:, 0:NE], in0=es[:, 0:NE], scalar1=m8[:, j:j + 1], scalar2=None, op0=ALU.is_equal), [r_es, r_m8], [r_oh])
                D_(lambda e: e.tensor_tensor(out=oh[:, 0:NE], in0=oh[:, 0:NE], in1=IO[:, 0:NE], op=ALU.mult), [r_oh, r_IO], [r_oh])
                D_(lambda e: e.tensor_reduce(out=sm[:, 11 + j:12 + j], in_=oh[:, 0:NE], axis=AX.X, op=ALU.add), [r_oh], [r_sm])
                D_(lambda e: e.scalar_tensor_tensor(out=ro[:, j:j + 1], in0=sm[:, 4:5], scalar=float(NE), in1=sm[:, 11 + j:12 + j], op0=ALU.mult, op1=ALU.add),
                   [r_sm], [r_ro])
            c.dma("pool", rto[tb * TBLK + sub * 128: tb * TBLK + (sub + 1) * 128, :], ro[:], [r_ro], [r_rto])
    c.finish([r_x1o, r_h2o, r_rto])
    return c


def build_s4(D, FE, NEL, C):
    KC = D // 128
    FT = FE // 128
    c = Ctx()
    IN = lambda n, s, dt=F32: c.dram(n, s, dt, kind="ExternalInput")
    xg, r_in = IN("xg", [NEL, KC, 128, C]); ws, _ = IN("ws", [NEL, 128, C])
    wg, _ = IN("wg", [NEL * FT, 128, KC * 128]); wu, _ = IN("wu", [NEL * FT, 128, KC * 128]); wd, _ = IN("wd", [NEL * KC, 128, FT * 128])
    yo, r_yo = c.dram("yo", [NEL, KC, 128, C], kind="ExternalOutput")
    blocks = [(b0, min(512, C - b0)) for b0 in range(0, C, 512)]
    xs, r_xs = c.sb("xs", [128, KC, 512]); act, r_act = c.sb("act", [128, FT, 512]); wsl, r_wsl = c.sb("wsl", [128, 512])
    wgs = [c.sb(f"wg{i}", [128, KC, 128]) for i in range(2)]; wus = [c.sb(f"wu{i}", [128, KC, 128]) for i in range(2)]
    wds = [c.sb(f"wd{i}", [128, FT, 128]) for i in range(2)]
    sg, r_sg = c.sb("sg", [128, 512]); yt = [c.sb(f"yt{i}", [128, 512]) for i in range(2)]
    psg = [c.ps(f"psg{i}", [128, 512]) for i in range(2)]; psu = [c.ps(f"psu{i}", [128, 512]) for i in range(2)]
    psy = [c.ps(f"psy{i}", [128, 512]) for i in range(2)]
    n = 0
    for e_ in range(NEL):
        for (b0, w) in blocks:
            c.dma("sp", xs[:, :, 0:w], xg[e_].rearrange("k p c -> p k c")[:, :, b0:b0 + w], [r_in], [r_xs])
            c.dma("sp", wsl[:, 0:w], ws[e_][:, b0:b0 + w], [r_in], [r_wsl])
            for f in range(FT):
                g_, r_g = wgs[n % 2]; u_, r_u = wus[n % 2]
                c.dma("sp", g_[:].rearrange("p a b -> p (a b)"), wg[e_ * FT + f], [r_in], [r_g])
                c.dma("sp", u_[:].rearrange("p a b -> p (a b)"), wu[e_ * FT + f], [r_in], [r_u])
                pg, r_pg = psg[n % 2]; pu, r_pu = psu[n % 2]
                for k in range(KC):
                    mm(c, pg[:, 0:w], g_[:, k, :], xs[:, k, 0:w], k == 0, k == KC - 1, [r_g, r_xs], [r_pg])
                for k in range(KC):
                    mm(c, pu[:, 0:w], u_[:, k, :], xs[:, k, 0:w], k == 0, k == KC - 1, [r_u, r_xs], [r_pu])
                c.op("act", lambda e: e.activation(out=sg[:, 0:w], in_=pg[:, 0:w], func=AF.Silu), [r_pg], [r_sg])
                c.op("dve", lambda e: e.tensor_tensor(out=act[:, f, 0:w], in0=pu[:, 0:w], in1=sg[:, 0:w], op=ALU.mult), [r_pu, r_sg], [r_act])
                n += 1
            for m in range(KC):
                d_, r_d = wds[m % 2]
                c.dma("sp", d_[:].rearrange("p a b -> p (a b)"), wd[e_ * KC + m], [r_in], [r_d])
                py, r_py = psy[m % 2]
                for f in range(FT):
                    mm(c, py[:, 0:w], d_[:, f, :], act[:, f, 0:w], f == 0, f == FT - 1, [r_d, r_act], [r_py])
                y_, r_y = yt[m % 2]
                c.op("dve", lambda e: e.tensor_tensor(out=y_[:, 0:w], in0=py[:, 0:w], in1=wsl[:, 0:w], op=ALU.mult), [r_py, r_wsl], [r_y])
                c.dma("pool", yo[e_, m][:, b0:b0 + w], y_[:, 0:w], [r_y], [r_yo])
    c.finish([r_yo])
    return c


def build_s5(D, NT, TBLK=512):
    KC = D // 128
    NTB = NT // TBLK
    c = Ctx()
    IN = lambda n, s, dt=F32: c.dram(n, s, dt, kind="ExternalInput")
    x1, r_in = IN("x1", [KC, 128, NT]); y1, _ = IN("y1", [KC, 128, NT]); y2, _ = IN("y2", [KC, 128, NT]); vecs, _ = IN("vecs", [128, 2 * KC])
    oo, r_oo = c.dram("oo", [KC, 128, NT], kind="ExternalOutput")
    V, r_V = c.sb("V", [128, 2 * KC])
    c.dma("sp", V[:], vecs, [r_in], [r_V])
    ones, r_ones = c.sb("ones", [128, 128])
    c.op("dve", lambda e: e.memset(ones[:], 1.0), [], [r_ones])
    z, r_z = c.sb("z", [128, KC, TBLK]); rstd, r_rstd = c.sb("rstd", [128, TBLK])
    ld = [[c.sb(f"ld{i}_{j}", [128, TBLK]) for j in range(3)] for i in range(2)]
    sq = [c.sb(f"sq{i}", [128, TBLK]) for i in range(2)]; ot = [c.sb(f"ot{i}", [128, TBLK]) for i in range(2)]
    pss, r_pss = c.ps("pss", [128, 512])
    for tb in range(NTB):
        ts = slice(tb * TBLK, (tb + 1) * TBLK)
        for m in range(KC):
            (a, r_a), (b, r_b), (x, r_x) = ld[m % 2]
            c.dma("sp", a[:], y1[m][:, ts], [r_in], [r_a]); c.dma("sp", b[:], y2[m][:, ts], [r_in], [r_b]); c.dma("sp", x[:], x1[m][:, ts], [r_in], [r_x])
            c.op("pool", lambda e: e.tensor_tensor(out=a[:], in0=a[:], in1=b[:], op=ALU.add), [r_a, r_b], [r_a])
            c.op("dve", lambda e: e.scalar_tensor_tensor(out=z[:, m, :], in0=a[:], scalar=V[:, m:m + 1], in1=x[:], op0=ALU.mult, op1=ALU.add),
                 [r_a, r_V, r_x], [r_z])
            s_, r_s = sq[m % 2]
            c.op("act", lambda e: e.activation(out=s_[:], in_=z[:, m, :], func=AF.Square), [r_z], [r_s])
            mm(c, pss[:, 0:TBLK], ones[:], s_[:], m == 0, m == KC - 1, [r_ones, r_s], [r_pss])
        c.op("dve", lambda e: e.tensor_scalar(out=rstd[:], in0=pss[:, 0:TBLK], scalar1=1.0 / D, scalar2=EPS, op0=ALU.mult, op1=ALU.add), [r_pss], [r_rstd])
        c.op("act", lambda e: e.activation(out=rstd[:], in_=rstd[:], func=AF.Sqrt), [r_rstd], [r_rstd])
        c.op("dve", lambda e: e.reciprocal(out=rstd[:], in_=rstd[:]), [r_rstd], [r_rstd])
        for m in range(KC):
            o_, r_o = ot[m % 2]
            c.op("dve", lambda e: e.scalar_tensor_tensor(out=o_[:], in0=z[:, m, :], scalar=V[:, KC + m:KC + m + 1], in1=rstd[:], op0=ALU.mult, op1=ALU.mult),
                 [r_z, r_V, r_rstd], [r_o])
            c.dma("pool", oo[m][:, ts], o_[:], [r_o], [r_oo])
    c.finish([r_oo])
    return c


def kernel(x, c, positions, w_ada, b_ada, g_norm1, w_in, lam_q1, lam_k1, lam_q2, lam_k2, g_subln,
           w_proj_a, w_proj_b, w_out, g_norm2, router_g, router_g_b, router_e, router_e_b,
           w_gate, w_up, w_down, g_final):
    f = lambda a: np.asarray(a, dtype=np.float32)
    x = f(x); B, S, D = x.shape
    KC = D // 128
    T = B * S
    NT = T // NCORES
    HA, HKV, HB, HI = 16, 4, 8, 16
    NG, NE = 4, 8
    FE = w_gate.shape[-1]
    x2d = x.reshape(T, D)
    mod = run_s0(f(c), f(w_ada)[0], f(b_ada)[0])
    shift1, scale1, gate1, shift2, scale2, gate2 = np.split(mod, 6, axis=1)
    projT = run_s1(x2d, f(w_in)[0], f(g_norm1)[0], scale1, shift1, TPB=S // 512)
    QB = S // 4
    ctx = build_s2(S, QB, HA, HKV, HB, HI, min(256, S // 4), 0.2)
    pos = np.asarray(positions).astype(np.int32)
    lam4 = [f(lam_q1)[0], f(lam_k1)[0], f(lam_q2)[0], f(lam_k2)[0]]
    maps, qidxs = [], []
    for r in range(NCORES):
        b, hf = divmod(r, 2)
        m, qidx = s2_map(projT[:, b * S:(b + 1) * S], pos[b], hf, S, QB, HA, HKV, HB, HI, lam4, f(g_subln)[0])
        maps.append(m); qidxs.append(qidx)
    res = ctx.run(maps)
    o_a = np.zeros((T, HA * 128), np.float32); o_b = np.zeros((T, HB * 256), np.float32)
    for r in range(NCORES):
        b = r // 2
        o_a[b * S + qidxs[r]] = res[r]["oa"]; o_b[b * S + qidxs[r]] = res[r]["ob"]
    del maps, res
    off_g = HA * 128 + 2 * HKV * 128 + HI * 64 + 64 + HI + 4 * HB * 128 + HB * 256
    NR = NG + NG * NE
    wr_full = np.concatenate([f(router_g)[0], f(router_e)[0].transpose(1, 0, 2).reshape(D, NG * NE)], axis=1)
    rbias = np.concatenate([f(router_g_b)[0], f(router_e_b)[0].reshape(-1)])
    wa_t = tile_w(f(w_proj_a)[0], (HA * 128) // 128); wb_t = tile_w(f(w_proj_b)[0], (HB * 256) // 128); wo_t = tile_w(f(w_out)[0], KC)
    ctx = build_s3(D, HA * 128, NT, NG, NE)
    maps = []
    for r in range(NCORES):
        b = r // 2
        ts = slice(r * NT, (r + 1) * NT)
        maps.append({
            "oaT": tile_xT(o_a[ts], 256), "obT": tile_xT(o_b[ts], 256),
            "gaT": np.ascontiguousarray(projT[off_g:off_g + D, ts]).reshape(KC, 128, NT),
            "gbT": np.ascontiguousarray(projT[off_g + D:off_g + 2 * D, ts]).reshape(KC, 128, NT),
            "xT": np.ascontiguousarray(x2d[ts].T).reshape(KC, 128, NT),
            "wa": wa_t, "wb": wb_t, "wo": wo_t,
            "vecs": np.concatenate([vec_pk(gate1[b], KC), vec_pk(f(g_norm2)[0], KC), vec_pk(scale2[b], KC), vec_pk(shift2[b], KC)], axis=1),
            "wr": np.ascontiguousarray(wr_full.reshape(KC, 128, NR).transpose(1, 0, 2)).reshape(128, KC * NR),
            "rb": np.ascontiguousarray(np.broadcast_to(rbias[None, :], (128, NR))),
            "iot": np.ascontiguousarray(np.broadcast_to(np.arange(8, dtype=np.float32)[None, :], (128, 8))),
        })
    res = ctx.run(maps)
    x1T = [res[r]["x1o"] for r in range(NCORES)]
    h2 = np.concatenate([res[r]["h2o"].reshape(D, NT).T for r in range(NCORES)], axis=0)
    route = np.concatenate([res[r]["rto"] for r in range(NCORES)], axis=0)
    del maps, res, projT
    eid = np.rint(route[:, 0:2]).astype(np.int64)
    NEXP = NG * NE
    NEL = NEXP // NCORES
    lists = [[np.nonzero(eid[:, k] == e)[0] for k in range(2)] for e in range(NEXP)]
    cmax = max(len(l[0]) + len(l[1]) for l in lists)
    C = max(128, -(-cmax // 32) * 32)
    ctx = build_s4(D, FE, NEL, C)
    maps = []
    for r in range(NCORES):
        xg = np.zeros((NEL, KC, 128, C), np.float32); ws = np.zeros((NEL, 128, C), np.float32)
        wg_l, wu_l, wd_l = [], [], []
        for el in range(NEL):
            e = r * NEL + el
            toks = np.concatenate(lists[e]); n = len(toks)
            wsl = np.concatenate([route[lists[e][0], 2], route[lists[e][1], 3]])
            xg[el, :, :, :n] = h2[toks].T.reshape(KC, 128, n)
            ws[el, :, :n] = wsl[None, :]
            wg_l.append(tile_w(f(w_gate[0, e]), KC)); wu_l.append(tile_w(f(w_up[0, e]), KC)); wd_l.append(tile_w(f(w_down[0, e]), FE // 128))
        maps.append({"xg": xg, "ws": ws, "wg": np.concatenate(wg_l), "wu": np.concatenate(wu_l), "wd": np.concatenate(wd_l)})
    res = ctx.run(maps)
    ys = [np.zeros((T, D), np.float32), np.zeros((T, D), np.float32)]
    for r in range(NCORES):
        yo = res[r]["yo"]
        for el in range(NEL):
            e = r * NEL + el
            yT = yo[el].reshape(D, C)
            n0 = len(lists[e][0]); n1 = len(lists[e][1])
            ys[0][lists[e][0]] = yT[:, :n0].T
            ys[1][lists[e][1]] = yT[:, n0:n0 + n1].T
    del maps, res, h2
    ctx = build_s5(D, NT)
    maps = []
    for r in range(NCORES):
        b = r // 2
        ts = slice(r * NT, (r + 1) * NT)
        maps.append({"x1": x1T[r], "y1": np.ascontiguousarray(ys[0][ts].T).reshape(KC, 128, NT),
                     "y2": np.ascontiguousarray(ys[1][ts].T).reshape(KC, 128, NT),
                     "vecs": np.concatenate([vec_pk(gate2[b], KC), vec_pk(f(g_final), KC)], axis=1)})
    res = ctx.run(maps)
    out = np.concatenate([res[r]["oo"].reshape(D, NT).T for r in range(NCORES)], axis=0)
    return np.ascontiguousarray(out.reshape(B, S, D)).astype(np.float32)
```
